# Optimizing a Trainium2 kernel written in Bass

```python
import math
import jax, jax.numpy as jnp
from jax import lax
import numpy as np

D_MODEL = 2048
BATCH = 2
SEQ = 16384
DEPTH = 2

CTX_LEN = 256
GRID_W = 64
N_MIXERS = 4
GROUP_WIDTH = D_MODEL // N_MIXERS
HEAD_DIM = 128
NA_HEADS = GROUP_WIDTH // HEAD_DIM
NA_WIN_R = 8
NA_WIN_C = 16
DIFF_HEADS = GROUP_WIDTH // HEAD_DIM
DIFF_DIM = HEAD_DIM // 2
DIFF_Q_BLOCK = 128
RET_HEADS = GROUP_WIDTH // HEAD_DIM
RET_CHUNK = 128
SWA_Q_HEADS = GROUP_WIDTH // HEAD_DIM
SWA_KV_HEADS = SWA_Q_HEADS // 2
SWA_KV_WIDTH = SWA_KV_HEADS * HEAD_DIM
SWA_WINDOW = 128
SWA_BLOCK = 128
N_EXPERTS = 16
EXPERT_FF = D_MODEL // 2
EC_CAPACITY = 2
ROPE_BASE = 10000.0
NORM_EPS = 1e-6
GN_EPS = 1e-5
NEG_INF = -1e30
IN_SPLITS = (GROUP_WIDTH,) * 3 + (GROUP_WIDTH,) * 3 + (GROUP_WIDTH,) * 4 + (GROUP_WIDTH, SWA_KV_WIDTH, SWA_KV_WIDTH)
IN_WIDTH = sum(IN_SPLITS)
SPLIT_POINTS = tuple(int(v) for v in np.cumsum(IN_SPLITS)[:-1])

kernel_name = 'hybrid_diffusion_parallel_heads_ec_moe'


def rmsnorm(x, g):
    xf = x.astype(jnp.float32)
    y = xf * lax.rsqrt(jnp.mean(xf * xf, axis=-1, keepdims=True) + NORM_EPS)
    return (y * g.astype(jnp.float32)).astype(x.dtype)


def axial_rope_tables(n_tok, dim, dtype):
    t = jnp.arange(n_tok)
    row = (t // GRID_W).astype(jnp.float32)
    col = (t % GRID_W).astype(jnp.float32)
    nf = dim // 4
    inv = ROPE_BASE ** (-jnp.arange(nf, dtype=jnp.float32) / nf)
    ar = row[:, None, None] * inv
    ac = col[:, None, None] * inv
    return tuple(a.astype(dtype) for a in (jnp.cos(ar), jnp.sin(ar), jnp.cos(ac), jnp.sin(ac)))


def _rot_half(u, cos, sin):
    u1, u2 = jnp.split(u, 2, axis=-1)
    return jnp.concatenate([u1 * cos - u2 * sin, u2 * cos + u1 * sin], axis=-1)


def apply_axial_rope(x, tables):
    cos_r, sin_r, cos_c, sin_c = tables
    x_row, x_col = jnp.split(x, 2, axis=-1)
    return jnp.concatenate([_rot_half(x_row, cos_r, sin_r), _rot_half(x_col, cos_c, sin_c)], axis=-1)


def split_heads(t, n_heads):
    return t.reshape(t.shape[0], t.shape[1], n_heads, t.shape[-1] // n_heads)


def context_attention(q, k, v, sink=None):
    b, l, hq, d = q.shape
    hkv = k.shape[2]
    grp = hq // hkv
    qg = q.reshape(b, l, hkv, grp, d)
    s = jnp.einsum('bqkgd,bskd->bkgqs', qg, k).astype(jnp.float32) * (d ** -0.5)
    if sink is not None:
        s_sink = jnp.broadcast_to(sink.astype(jnp.float32).reshape(1, hkv, grp, 1, 1), s.shape[:-1] + (1,))
        p = jax.nn.softmax(jnp.concatenate([s, s_sink], axis=-1), axis=-1)[..., :-1]
    else:
        p = jax.nn.softmax(s, axis=-1)
    o = jnp.einsum('bkgqs,bskd->bqkgd', p.astype(v.dtype), v)
    return o.reshape(b, l, hq * d)


def neighbourhood_attention(q, k, v, kc, vc, rpb):
    b, n, h, d = q.shape
    rows = n // GRID_W
    kr = min(NA_WIN_R, rows)
    r = jnp.arange(rows)
    row_idx = jnp.clip(r - kr // 2, 0, rows - kr)[:, None] + jnp.arange(kr)[None, :]
    cols = jnp.arange(GRID_W)
    col_start = jnp.clip(cols - NA_WIN_C // 2, 0, GRID_W - NA_WIN_C)
    col_ok = (cols[None, :] >= col_start[:, None]) & (cols[None, :] < col_start[:, None] + NA_WIN_C)
    row_off = row_idx - r[:, None] + NA_WIN_R - 1
    col_off = jnp.clip(cols[None, :] - cols[:, None] + NA_WIN_C - 1, 0, 2 * NA_WIN_C - 2)
    bias = rpb.astype(jnp.float32)[:, row_off][:, :, :, col_off]
    bias = jnp.where(col_ok[None, None, None], bias, NEG_INF).transpose(0, 1, 3, 2, 4)
    qg = q.reshape(b, rows, GRID_W, h, d)
    kg = k.reshape(b, rows, GRID_W, h, d)[:, row_idx]
    vg = v.reshape(b, rows, GRID_W, h, d)[:, row_idx]
    scale = d ** -0.5
    s_loc = jnp.einsum('brqhd,brkchd->bhrqkc', qg, kg).astype(jnp.float32) * scale + bias[None]
    s_ctx = jnp.einsum('brqhd,blhd->bhrql', qg, kc).astype(jnp.float32) * scale
    nloc = kr * GRID_W
    logits = jnp.concatenate([s_loc.reshape(b, h, rows, GRID_W, nloc), s_ctx], axis=-1)
    p = jax.nn.softmax(logits, axis=-1).astype(v.dtype)
    p_loc = p[..., :nloc].reshape(b, h, rows, GRID_W, kr, GRID_W)
    o = jnp.einsum('bhrqkc,brkchd->brqhd', p_loc, vg) + jnp.einsum('bhrql,blhd->brqhd', p[..., nloc:], vc)
    return o.reshape(b, n, h * d)


def _diff_apply(q, k, v, lam):
    d = q.shape[-1]
    s = jnp.einsum('bqhmd,bshmd->bhmqs', q, k).astype(jnp.float32) * (d ** -0.5)
    p = jax.nn.softmax(s, axis=-1)
    w = p[:, :, 0] - lam * p[:, :, 1]
    return jnp.einsum('bhqs,bshe->bqhe', w.astype(v.dtype), v)


def _diff_post(o, g, lam_init):
    b, t, h, e = o.shape
    return (rmsnorm(o, g) * (1.0 - lam_init)).reshape(b, t, h * e)


def differential_attention(q, k, v, kc, vc, lam, lam_init, norm_g):
    b, n, h, _, d = q.shape
    k_all = jnp.concatenate([k, kc], axis=1)
    v_all = jnp.concatenate([v, vc], axis=1)
    nb = n // DIFF_Q_BLOCK
    q_blocks = jnp.swapaxes(q.reshape(b, nb, DIFF_Q_BLOCK, h, 2, d), 0, 1)
    o = lax.map(lambda qb: _diff_apply(qb, k_all, v_all, lam), q_blocks)
    o = jnp.swapaxes(o, 0, 1).reshape(b, n, h, 2 * d)
    return _diff_post(o, norm_g, lam_init)


def _retention_chunks(q, k, v, log_gamma, s0):
    b, t, h, d = q.shape
    nc = t // RET_CHUNK
    pos = jnp.arange(RET_CHUNK, dtype=jnp.float32)
    rel = pos[:, None] - pos[None, :]
    intra = jnp.where(rel >= 0, jnp.exp(jnp.maximum(rel, 0.0) * log_gamma[:, None, None]), 0.0)
    q_dec = jnp.exp((pos + 1.0) * log_gamma[:, None])[..., None]
    k_dec = jnp.exp((RET_CHUNK - 1.0 - pos) * log_gamma[:, None])[..., None]
    c_dec = jnp.exp(RET_CHUNK * log_gamma)[:, None, None]

    def to_chunks(a):
        return a.astype(jnp.float32).reshape(b, nc, RET_CHUNK, h, d).transpose(1, 0, 3, 2, 4)

    def step(s, qkv):
        qi, ki, vi = qkv
        a = jnp.einsum('bhnd,bhmd->bhnm', qi, ki) * intra
        o = jnp.einsum('bhnm,bhme->bhne', a, vi) + jnp.einsum('bhnd,bhde->bhne', qi * q_dec, s)
        s = c_dec * s + jnp.einsum('bhmd,bhme->bhde', ki * k_dec, vi)
        return s, o

    s_fin, o = lax.scan(step, s0, (to_chunks(q), to_chunks(k), to_chunks(v)))
    return o.transpose(1, 0, 3, 2, 4).reshape(b, t, h, d), s_fin


def _retention_post(o, gate, gn_g, gn_b):
    b, t, h, d = o.shape
    mu = jnp.mean(o, axis=-1, keepdims=True)
    var = jnp.mean(jnp.square(o - mu), axis=-1, keepdims=True)
    y = ((o - mu) * lax.rsqrt(var + GN_EPS)).reshape(b, t, h * d)
    y = y * gn_g.astype(jnp.float32) + gn_b.astype(jnp.float32)
    return (jax.nn.silu(gate.astype(jnp.float32)) * y).astype(gate.dtype)


def bidirectional_retention(q, k, v, gate, qc, kc, vc, decay_f, decay_b, gn_g, gn_b):
    b, _, h, d = q.shape
    lg_f = jax.nn.log_sigmoid(decay_f.astype(jnp.float32))
    lg_b = jax.nn.log_sigmoid(decay_b.astype(jnp.float32))
    s0 = jnp.zeros((b, h, d, d), jnp.float32)
    rev = lambda a: jnp.flip(a, axis=1)
    oc_f, s_f = _retention_chunks(qc, kc, vc, lg_f, s0)
    oc_b, s_b = _retention_chunks(rev(qc), rev(kc), rev(vc), lg_b, s0)
    o_f, _ = _retention_chunks(q, k, v, lg_f, s_f)
    o_b, _ = _retention_chunks(rev(q), rev(k), rev(v), lg_b, s_b)
    return (_retention_post(o_f + rev(o_b), gate, gn_g, gn_b), oc_f + rev(oc_b))


def window_gqa_sink(q, k, v, kc, vc, sink):
    b, n, hq, d = q.shape
    hkv = k.shape[2]
    grp = hq // hkv
    nb = n // SWA_BLOCK

    def band(a):
        ab = jnp.pad(a.reshape(b, nb, SWA_BLOCK, hkv, d), ((0, 0), (1, 1), (0, 0), (0, 0), (0, 0)))
        return jnp.concatenate([ab[:, :-2], ab[:, 1:-1], ab[:, 2:]], axis=2)

    kw, vw = band(k), band(v)
    qb = q.reshape(b, nb, SWA_BLOCK, hkv, grp, d)
    i = jnp.arange(SWA_BLOCK)[:, None]
    j = jnp.arange(3 * SWA_BLOCK)[None, :]
    kpos = (jnp.arange(nb)[:, None, None] - 1) * SWA_BLOCK + j[None]
    ok = (jnp.abs((j - SWA_BLOCK) - i)[None] <= SWA_WINDOW) & (kpos >= 0) & (kpos < n)
    scale = d ** -0.5
    s_loc = jnp.einsum('bnqkgd,bnskd->bnkgqs', qb, kw).astype(jnp.float32) * scale
    s_loc = jnp.where(ok[None, :, None, None], s_loc, NEG_INF)
    s_ctx = jnp.einsum('bnqkgd,blkd->bnkgql', qb, kc).astype(jnp.float32) * scale
    s_sink = jnp.broadcast_to(sink.astype(jnp.float32).reshape(1, 1, hkv, grp, 1, 1), s_ctx.shape[:-1] + (1,))
    nloc = 3 * SWA_BLOCK
    p = jax.nn.softmax(jnp.concatenate([s_loc, s_ctx, s_sink], axis=-1), axis=-1).astype(v.dtype)
    o = jnp.einsum('bnkgqs,bnskd->bnqkgd', p[..., :nloc], vw) + jnp.einsum('bnkgql,blkd->bnqkgd', p[..., nloc:-1], vc)
    return o.reshape(b, n, hq * d)


def expert_choice_ffn(h, w_router, w_gate, w_up, w_down):
    b, t, dm = h.shape
    cap = EC_CAPACITY * t // N_EXPERTS
    aff = jax.nn.softmax((h @ w_router).astype(jnp.float32), axis=-1)
    gate, idx = lax.top_k(jnp.swapaxes(aff, 1, 2), cap)
    xin = jax.vmap(lambda hb, ib: hb[ib])(h, idx)
    a = jnp.einsum('becd,edf->becf', xin, w_gate)
    u = jnp.einsum('becd,edf->becf', xin, w_up)
    y = jnp.einsum('becf,efd->becd', jax.nn.silu(a) * u, w_down) * gate[..., None].astype(h.dtype)
    return jax.vmap(lambda yb, ib: jnp.zeros((t, dm), yb.dtype).at[ib.reshape(-1)].add(yb.reshape(-1, dm)))(y, idx)


def hybrid_layer(x, xc, c, c_ctx, rope_d, rope_h, layer_idx, with_ctx,
                 w_mod, b_mod, norm1_g, w_in, na_rpb, diff_lambda, diff_norm_g,
                 ret_decay_fwd, ret_decay_bwd, ret_gn_g, ret_gn_b, swa_sink, w_out,
                 norm2_g, w_router, w_gate, w_up, w_down):
    b, n, _ = x.shape
    lc = xc.shape[1]
    mod = jax.nn.silu(c) @ w_mod + b_mod
    mod_c = jax.nn.silu(c_ctx) @ w_mod + b_mod
    sh1, sc1, g1, sh2, sc2, g2 = jnp.split(mod[:, None, :], 6, axis=-1)
    sh1c, sc1c, g1c, sh2c, sc2c, g2c = jnp.split(mod_c, 6, axis=-1)
    h = rmsnorm(x, norm1_g) * (1.0 + sc1) + sh1
    hc = rmsnorm(xc, norm1_g) * (1.0 + sc1c) + sh1c
    pl = jnp.split(h @ w_in, SPLIT_POINTS, axis=-1)
    pc = jnp.split(hc @ w_in, SPLIT_POINTS, axis=-1)

    qa, ka, va = [split_heads(t, NA_HEADS) for t in pl[0:3]]
    qac, kac, vac = [split_heads(t, NA_HEADS) for t in pc[0:3]]
    o_a = neighbourhood_attention(qa, ka, va, kac, vac, na_rpb)

    qb = apply_axial_rope(split_heads(pl[3], 2 * DIFF_HEADS), rope_d).reshape(b, n, DIFF_HEADS, 2, DIFF_DIM)
    kb = apply_axial_rope(split_heads(pl[4], 2 * DIFF_HEADS), rope_d).reshape(b, n, DIFF_HEADS, 2, DIFF_DIM)
    vb = split_heads(pl[5], DIFF_HEADS)
    qbc = pc[3].reshape(b, lc, DIFF_HEADS, 2, DIFF_DIM)
    kbc = pc[4].reshape(b, lc, DIFF_HEADS, 2, DIFF_DIM)
    vbc = split_heads(pc[5], DIFF_HEADS)
    lam_init = 0.8 - 0.6 * math.exp(-0.3 * layer_idx)
    lq1, lk1, lq2, lk2 = diff_lambda.astype(jnp.float32)
    lam = jnp.exp(jnp.sum(lq1 * lk1)) - jnp.exp(jnp.sum(lq2 * lk2)) + lam_init
    o_b = differential_attention(qb, kb, vb, kbc, vbc, lam, lam_init, diff_norm_g)

    ret_scale = HEAD_DIM ** -0.5
    q_ret = apply_axial_rope(split_heads(pl[6], RET_HEADS), rope_h)
    k_ret = apply_axial_rope(split_heads(pl[7], RET_HEADS), rope_h) * ret_scale
    v_ret = split_heads(pl[8], RET_HEADS)
    q_retc = split_heads(pc[6], RET_HEADS)
    k_retc = split_heads(pc[7], RET_HEADS) * ret_scale
    v_retc = split_heads(pc[8], RET_HEADS)
    o_c, ret_ctx = bidirectional_retention(q_ret, k_ret, v_ret, pl[9], q_retc, k_retc, v_retc,
                                           ret_decay_fwd, ret_decay_bwd, ret_gn_g, ret_gn_b)

    qd = apply_axial_rope(split_heads(pl[10], SWA_Q_HEADS), rope_h)
    kd = apply_axial_rope(split_heads(pl[11], SWA_KV_HEADS), rope_h)
    vd = split_heads(pl[12], SWA_KV_HEADS)
    qdc = split_heads(pc[10], SWA_Q_HEADS)
    kdc = split_heads(pc[11], SWA_KV_HEADS)
    vdc = split_heads(pc[12], SWA_KV_HEADS)
    o_d = window_gqa_sink(qd, kd, vd, kdc, vdc, swa_sink)

    x = x + g1 * (jnp.concatenate([o_a, o_b, o_c, o_d], axis=-1) @ w_out)
    h2 = rmsnorm(x, norm2_g) * (1.0 + sc2) + sh2
    x = x + g2 * expert_choice_ffn(h2, w_router, w_gate, w_up, w_down)

    if with_ctx:
        oc = jnp.concatenate([
            context_attention(qac, kac, vac),
            _diff_post(_diff_apply(qbc, kbc, vbc, lam), diff_norm_g, lam_init),
            _retention_post(ret_ctx, pc[9], ret_gn_g, ret_gn_b),
            context_attention(qdc, kdc, vdc, swa_sink)], axis=-1)
        xc = xc + g1c * (oc @ w_out)
        h2c = rmsnorm(xc, norm2_g) * (1.0 + sc2c) + sh2c
        xc = xc + g2c * expert_choice_ffn(h2c, w_router, w_gate, w_up, w_down)
    return x, xc


def setup_inputs(seed: int = 0) -> dict:
    key = jax.random.key(seed)
    ks = jax.random.split(key, 23)
    D = D_MODEL

    def nrm(k, shape, scale):
        return jax.random.normal(k, shape, jnp.float32) * scale

    ret_base = jnp.log(2.0 ** (5.0 + jnp.arange(RET_HEADS, dtype=jnp.float32)) - 1.0)
    return {
        'x': nrm(ks[0], (BATCH, SEQ, D), 1.0),
        'c': nrm(ks[1], (BATCH, D), 1.0),
        'ctx': nrm(ks[2], (BATCH, CTX_LEN, D), 1.0),
        'c_ctx': nrm(ks[3], (D,), 1.0),
        'w_mod': nrm(ks[4], (DEPTH, D, 6 * D), 0.5 * D ** -0.5),
        'b_mod': nrm(ks[5], (DEPTH, 6 * D), 0.02),
        'norm1_g': 1.0 + nrm(ks[6], (DEPTH, D), 0.05),
        'w_in': nrm(ks[7], (DEPTH, D, IN_WIDTH), D ** -0.5),
        'na_rpb': nrm(ks[8], (DEPTH, NA_HEADS, 2 * NA_WIN_R - 1, 2 * NA_WIN_C - 1), 0.1),
        'diff_lambda': nrm(ks[9], (DEPTH, 4, DIFF_DIM), 0.1),
        'diff_norm_g': 1.0 + nrm(ks[10], (DEPTH, 2 * DIFF_DIM), 0.05),
        'ret_decay_fwd': ret_base + nrm(ks[11], (DEPTH, RET_HEADS), 0.1),
        'ret_decay_bwd': ret_base + nrm(ks[12], (DEPTH, RET_HEADS), 0.1),
        'ret_gn_g': 1.0 + nrm(ks[13], (DEPTH, GROUP_WIDTH), 0.05),
        'ret_gn_b': nrm(ks[14], (DEPTH, GROUP_WIDTH), 0.02),
        'swa_sink': nrm(ks[15], (DEPTH, SWA_Q_HEADS), 0.5),
        'w_out': nrm(ks[16], (DEPTH, D, D), D ** -0.5),
        'norm2_g': 1.0 + nrm(ks[17], (DEPTH, D), 0.05),
        'w_router': nrm(ks[18], (DEPTH, D, N_EXPERTS), D ** -0.5),
        'w_gate': nrm(ks[19], (DEPTH, N_EXPERTS, D, EXPERT_FF), D ** -0.5),
        'w_up': nrm(ks[20], (DEPTH, N_EXPERTS, D, EXPERT_FF), D ** -0.5),
        'w_down': nrm(ks[21], (DEPTH, N_EXPERTS, EXPERT_FF, D), EXPERT_FF ** -0.5),
        'final_norm_g': 1.0 + nrm(ks[22], (D,), 0.05),
    }


def reference(x, c, ctx, c_ctx, w_mod, b_mod, norm1_g, w_in, na_rpb, diff_lambda, diff_norm_g,
              ret_decay_fwd, ret_decay_bwd, ret_gn_g, ret_gn_b, swa_sink, w_out, norm2_g,
              w_router, w_gate, w_up, w_down, final_norm_g):
    n = x.shape[1]
    rope_d = axial_rope_tables(n, DIFF_DIM, x.dtype)
    rope_h = axial_rope_tables(n, HEAD_DIM, x.dtype)
    xc = ctx
    for li in range(DEPTH):
        x, xc = hybrid_layer(x, xc, c, c_ctx, rope_d, rope_h, li, li < DEPTH - 1,
                             w_mod[li], b_mod[li], norm1_g[li], w_in[li], na_rpb[li], diff_lambda[li],
                             diff_norm_g[li], ret_decay_fwd[li], ret_decay_bwd[li], ret_gn_g[li], ret_gn_b[li],
                             swa_sink[li], w_out[li], norm2_g[li], w_router[li], w_gate[li], w_up[li], w_down[li])
    return rmsnorm(x, final_norm_g)
```

```python
import math
import numpy as np
from contextlib import ExitStack
import concourse.bass as bass
import concourse.mybir as mybir
from concourse.bass_utils import run_bass_kernel_spmd

F32 = mybir.dt.float32
BF16 = mybir.dt.bfloat16
I32 = mybir.dt.int32
AF = mybir.ActivationFunctionType
ALU = mybir.AluOpType
AX = mybir.AxisListType

NORM_EPS = 1e-6
GN_EPS = 1e-5
NEG_INF = -1e30
ROPE_BASE = 10000.0
GRID_W = 64


def dsl(v, n):
    if isinstance(v, (int, np.integer)):
        return slice(int(v), int(v) + n)
    return bass.ds(v, n)


class StopBuild(Exception):
    pass


class Cfg:
    def __init__(self, D=2048, SEQ=16384, CTX=256, NE=16, L=2):
        self.D, self.SEQ, self.CTX, self.NE, self.L = D, SEQ, CTX, NE, L
        self.GW = D // 4
        self.H = self.GW // 128
        self.HKV = self.H // 2
        self.FF = D // 2
        self.KC = D // 128
        self.FC = self.FF // 128
        self.INW = 11 * self.GW + 2 * (self.GW // 2)
        self.T = SEQ + CTX
        self.NTL = SEQ // 128
        self.NTC = CTX // 128
        self.NT = self.NTL + self.NTC
        self.CAP = 2 * SEQ // NE
        self.CAPC = 2 * CTX // NE
        self.CAPT = self.CAP + self.CAPC
        GW = self.GW
        names = ['qa', 'ka', 'va', 'qb', 'kb', 'vb', 'qc', 'kc', 'vc', 'gc', 'qd', 'kd', 'vd']
        widths = [GW] * 11 + [GW // 2] * 2
        ropes = [0, 0, 0, 64, 64, 0, 128, 128, 0, 0, 128, 128, 0]
        self.chunks = []
        c0 = 0
        for n, w, r in zip(names, widths, ropes):
            self.chunks.append((n, c0, w, r))
            c0 += w
        self.col = {n: c for (n, c, w, r) in self.chunks}
        H = self.H
        self.ptbase = {'qa': 0, 'ka': H, 'qb': 2 * H, 'kb': 3 * H, 'qc': 4 * H, 'kc': 5 * H, 'qd': 6 * H, 'kd': 7 * H}
        self.NQK = 7 * H + self.HKV
        self.NTYPE = 5
        self.stop = None
        self.dbg_layer = 0


class Sched:
    NS_DMA = 8

    def __init__(self, nc, es):
        self.nc = nc
        self.es = es
        self.engs = {'pe': nc.tensor, 'act': nc.scalar, 'dve': nc.vector, 'pool': nc.gpsimd, 'sp': nc.sync}
        self.csem = {}
        self.ccnt = {}
        self.semobj = {}
        for e in ('pe', 'act', 'dve', 'pool'):
            n = f"c_{e}"
            s = es.enter_context(nc.semaphore(n))
            self.csem[e] = (n, s)
            self.semobj[n] = s
            self.ccnt[e] = 0
        self.dsem = {}
        self.dcnt = {}
        for q in ('sp', 'pool', 'act'):
            self.dsem[q] = []
            for i in range(self.NS_DMA):
                n = f"d_{q}_{i}"
                s = es.enter_context(nc.semaphore(n))
                self.semobj[n] = s
                self.dsem[q].append(n)
            self.dcnt[q] = 0
        self.known = {e: {} for e in self.engs}
        self.res = {}
        self.ninst = 0

    def _wait(self, e, ev):
        if ev is None:
            return
        name, val = ev
        k = self.known[e]
        if k.get(name, 0) >= val:
            return
        self.engs[e].wait_ge(self.semobj[name], val)
        self.ninst += 1
        k[name] = val

    def _deps(self, e, reads, writes, same_ok=False):
        evs = []
        for r in reads:
            st = self.res.get(r)
            if st and st['w']:
                evs.append(st['w'])
        for w in writes:
            st = self.res.get(w)
            if st:
                if st['w']:
                    evs.append(st['w'])
                evs.extend(st['r'])
        best = {}
        for (n, v, src) in evs:
            if same_ok and src == e:
                continue
            if best.get(n, 0) < v:
                best[n] = v
        for n, v in best.items():
            self._wait(e, (n, v))

    def _record(self, e, ev, reads, writes):
        n, v = ev
        rec = (n, v, e)
        for r in reads:
            st = self.res.setdefault(r, {'w': None, 'r': []})
            st['r'].append(rec)
            if len(st['r']) > 48:
                best = {}
                for (nn, vv, ss) in st['r']:
                    if best.get(nn, (0, None))[0] < vv:
                        best[nn] = (vv, ss)
                st['r'] = [(nn, vv, ss) for nn, (vv, ss) in best.items()]
        for w in writes:
            self.res[w] = {'w': rec, 'r': []}

    def op(self, e, fn, reads=(), writes=(), same_ok=False):
        self._deps(e, reads, writes, same_ok=same_ok)
        inst = fn(self.engs[e])
        name, sem = self.csem[e]
        self.ccnt[e] += 1
        inst.then_inc(sem, 1)
        self._record(e, (name, self.ccnt[e]), reads, writes)
        self.ninst += 1
        return inst

    def dma(self, q, fn, reads=(), writes=()):
        self._deps(q, reads, writes)
        i = self.dcnt[q]
        self.dcnt[q] += 1
        name = self.dsem[q][i % self.NS_DMA]
        val = 16 * (i // self.NS_DMA + 1)
        if val > 16:
            self._wait(q, (name, val - 16))
        inst = fn(self.engs[q])
        inst.then_inc(self.semobj[name], 16)
        self._record(q, (name, val), reads, writes)
        self.ninst += 1
        return inst

    def sync_all(self):
        nc = self.nc
        evs = []
        for e, (name, sem) in self.csem.items():
            if self.ccnt[e] > 0:
                evs.append((name, self.ccnt[e]))
        for q, names in self.dsem.items():
            cnt = self.dcnt[q]
            for j, name in enumerate(names):
                k = (cnt - j + self.NS_DMA - 1) // self.NS_DMA
                if k > 0:
                    evs.append((name, 16 * k))
        for ev in evs:
            self._wait('sp', ev)
        nc.all_engine_barrier()
        for name, sem in self.semobj.items():
            if name.startswith('d_pool_'):
                continue
            nc.sync.sem_clear(sem)
        nc.all_engine_barrier()
        self.ninst += 2 + len(self.semobj)
        for e in self.ccnt:
            self.ccnt[e] = 0
        for q in self.dcnt:
            if q != 'pool':
                self.dcnt[q] = 0
        self.known = {e: {} for e in self.engs}
        self.res = {}

    def loop(self, start, end, body):
        if end <= start:
            return
        if end - start == 1:
            body(start)
            return
        self.sync_all()
        with self.fori(start, end) as i:
            body(i)
            self.sync_all()

    def fori(self, start, end):
        from contextlib import contextmanager
        nc = self.nc
        if not hasattr(self, 'loop_regs'):
            self.loop_regs = []
            self.loop_depth = 0

        if not hasattr(self, 'reg_pfx'):
            self.reg_pfx = []
            self.freed = set()
            for en in self.engs.values():
                r = en.alloc_register("mkprobe")
                self.reg_pfx.append((en, r.name[:-len("mkprobe")], r.engine))
                en.free_register(r)

        @contextmanager
        def _loop():
            d = self.loop_depth
            id0 = nc.next_id()
            while len(self.loop_regs) <= d:
                self.loop_regs.append(nc.alloc_registers(f"mkloop{len(self.loop_regs)}", engines=mybir.ALL_ENGINES))
            registers = self.loop_regs[d]
            self.loop_depth += 1
            lid = nc.next_id()
            loop_start = f"mk_fori_{lid}_loop"
            loop_end = f"mk_fori_{lid}_end"
            nc.regs_mov(registers, start)
            nc.br(loop_start, engines=mybir.ALL_ENGINES)
            with nc.body(loop_start, valid_engines=mybir.ALL_ENGINES):
                yield nc.snap(registers, min_val=start, max_val=end - 1)
                nc.regs_alu(registers, registers, 1, op=mybir.AluOpType.add)
                nc.br_lt(registers, end, on_true=loop_start, on_false=loop_end, engines=mybir.ALL_ENGINES)
            nc.switch_bb(loop_end)
            self.loop_depth -= 1
            id1 = nc.next_id()
            for (en, pfx, et) in self.reg_pfx:
                for k in range(id0, id1):
                    for nm in (f"{pfx}tmp_{k}", f"{pfx}{pfx}mkloop{d}_snap_{k}"):
                        if nm in self.freed:
                            continue
                        try:
                            en.free_register(bass.RegisterHandle(nm, et))
                            self.freed.add(nm)
                        except Exception:
                            pass
        return _loop()


def build_program(cfg, debug=False):
    D, SEQ, CTX, NE, L = cfg.D, cfg.SEQ, cfg.CTX, cfg.NE, cfg.L
    GW, H, HKV, FF, KC, FC, INW = cfg.GW, cfg.H, cfg.HKV, cfg.FF, cfg.KC, cfg.FC, cfg.INW
    T, NTL, NTC, NT = cfg.T, cfg.NTL, cfg.NTC, cfg.NT
    CAP, CAPC, CAPT = cfg.CAP, cfg.CAPC, cfg.CAPT
    NQK, NTYPE = cfg.NQK, cfg.NTYPE
    ND = D // 512 if D >= 512 else 1
    NW = min(512, D)
    nc = bass.Bass("TRN2", target_bir_lowering=False)

    def din(name, shape, dt=F32):
        return nc.dram_tensor(name, list(shape), dt, kind="ExternalInput").ap()

    def dscr(name, shape, dt):
        return nc.dram_tensor(name, list(shape), dt, kind="Internal").ap()

    xin = din("xin", [T, D])
    cl = din("cl", [128, KC * 2])
    w_mod = din("w_mod", [L, D, 6 * D])
    b_mod = din("b_mod", [L, 6 * D])
    norm1_g = din("norm1_g", [L, D])
    w_in = din("w_in", [L, D, INW])
    nab = din("nab", [L, 128, NTYPE * H * 5 * 128])
    diff_lambda = din("diff_lambda", [L, 256])
    diff_norm_g = din("diff_norm_g", [L, 128])
    ret_decay_fwd = din("ret_decay_fwd", [L, H])
    ret_decay_bwd = din("ret_decay_bwd", [L, H])
    ret_gn_g = din("ret_gn_g", [L, GW])
    ret_gn_b = din("ret_gn_b", [L, GW])
    swa_sink = din("swa_sink", [L, H])
    w_out = din("w_out", [L, D, D])
    norm2_g = din("norm2_g", [L, D])
    w_router = din("w_router", [L, D, NE])
    w_gate = din("w_gate", [L, NE, D, FF])
    w_up = din("w_up", [L, NE, D, FF])
    w_down = din("w_down", [L, NE, FF, D])
    final_norm_g = din("final_norm_g", [1, D])
    rope = din("rope", [SEQ, 4 * GW])
    yout = nc.dram_tensor("y", [SEQ, D], F32, kind="ExternalOutput").ap()

    X = dscr("X", [T, D], F32)
    MODV = dscr("MODV", [2, 6 * D], F32)
    WINB = dscr("WINB", [D, INW], BF16)
    P = dscr("P", [T, INW], BF16)
    PT = dscr("PT", [NQK, 128, T], BF16)
    OT = dscr("OT", [4 * H, 128, T], BF16)
    OF = dscr("OF", [T, GW], F32)
    H2 = dscr("H2", [T, D], BF16)
    XEf = dscr("XE", [NE * CAPT, D], BF16)
    YEf = dscr("YE", [NE * CAPT, D], BF16)
    XE = XEf.rearrange("(e s) d -> e s d", e=NE)
    YE = YEf.rearrange("(e s) d -> e s d", e=NE)
    dbg = {}
    if debug:
        for nm, ap_, dt in (("P", P, BF16), ("PT", PT, BF16), ("OT", OT, BF16), ("X", X, F32), ("MODV", MODV, F32), ("H2", H2, BF16)):
            dbg[nm] = (ap_, nc.dram_tensor("dbg_" + nm, list(ap_.shape), dt, kind="ExternalOutput").ap())

    with ExitStack() as es:
        S = Sched(nc, es)

        def dma(out, in_, R, W, q='sp'):
            S.dma(q, lambda e: e.dma_start(out=out, in_=in_), reads=R, writes=W)

        ind_state = []

        def ind_dma(fn, bc, R, W):
            id0 = nc.next_id()
            S.dma('pool', fn, reads=R, writes=W)
            id1 = nc.next_id()
            if not ind_state:
                r = nc.gpsimd.alloc_register("mkp")
                ind_state.append((r.name[:-3], r.engine))
                nc.gpsimd.free_register(r)
            pfx, et = ind_state[0]
            for k in range(id0, id1 + 1):
                try:
                    nc.gpsimd.free_register(bass.RegisterHandle(f"{pfx}val_{bc}_{k}", et))
                except Exception:
                    pass

        def mm(out, lhsT, rhs, R, W, start=True, stop=True):
            S.op('pe', lambda e: e.matmul(out, lhsT=lhsT, rhs=rhs, start=start, stop=stop), reads=R, writes=W, same_ok=True)

        def tr(out, in_, ident, R, W):
            S.op('pe', lambda e: e.transpose(out=out, in_=in_, identity=ident), reads=R, writes=W, same_ok=True)

        def act(out, in_, func, R, W, **kw):
            S.op('act', lambda e: e.activation(out=out, in_=in_, func=func, **kw), reads=R, writes=W)

        def tt(out, a, b, op, R, W, eng='dve'):
            S.op(eng, lambda e: e.tensor_tensor(out=out, in0=a, in1=b, op=op), reads=R, writes=W)

        def tsc(out, a, s1, s2, op0, op1, R, W, eng='dve'):
            if s2 is None:
                S.op(eng, lambda e: e.tensor_scalar(out=out, in0=a, scalar1=s1, scalar2=None, op0=op0), reads=R, writes=W)
            else:
                S.op(eng, lambda e: e.tensor_scalar(out=out, in0=a, scalar1=s1, scalar2=s2, op0=op0, op1=op1), reads=R, writes=W)

        def stt(out, a, s, b, op0, op1, R, W, eng='dve'):
            S.op(eng, lambda e: e.scalar_tensor_tensor(out=out, in0=a, scalar=s, in1=b, op0=op0, op1=op1), reads=R, writes=W)

        def cp(out, in_, R, W, eng='dve'):
            if eng == 'act':
                act(out, in_, AF.Copy, R, W)
            else:
                S.op(eng, lambda e: e.tensor_copy(out=out, in_=in_), reads=R, writes=W)

        def memset(ap_, val, W, eng='dve'):
            S.op(eng, lambda e: e.memset(ap_, val), writes=W)

        def recip(out, in_, R, W):
            S.op('dve', lambda e: e.reciprocal(out=out, in_=in_), reads=R, writes=W)

        def rstd_from(ss, n, eps, tag):
            tsc(ss, ss, 1.0 / n, eps, ALU.mult, ALU.add, [tag], [tag])
            act(ss, ss, AF.Sqrt, [tag], [tag])
            recip(ss, ss, [tag], [tag])

        cs = ExitStack()
        es.enter_context(cs)

        uid = [0]

        def SB(stack, name, shape, dt=F32):
            uid[0] += 1
            return stack.enter_context(nc.sbuf_tensor(f"{name}_{uid[0]}", list(shape), dt))

        def PS(stack, name, shape, dt=F32):
            uid[0] += 1
            return stack.enter_context(nc.psum_tensor(f"{name}_{uid[0]}", list(shape), dt))

        REL = SB(cs, "REL", [128, 128])
        ident = SB(cs, "ident", [128, 128], BF16)
        onesf = SB(cs, "onesf", [128, 128])
        onesb = SB(cs, "onesb", [128, 128], BF16)
        TRIU = SB(cs, "TRIU", [128, 128], BF16)
        TRIL = SB(cs, "TRIL", [128, 128], BF16)
        tmpc = SB(cs, "tmpc", [128, 128])
        AFFT = SB(cs, "AFFT", [128, NT, NE])
        POSI = SB(cs, "POSI", [128, NT, NE], I32)
        WGT = SB(cs, "WGT", [128, NT, NE])
        S.op('pool', lambda e: e.iota(REL[:], pattern=[[1, 128]], base=0, channel_multiplier=-1,
                                      allow_small_or_imprecise_dtypes=True), writes=['REL'])
        tsc(tmpc[:], REL[:], 0.0, None, ALU.is_equal, None, ['REL'], ['tmpc'])
        cp(ident[:], tmpc[:], ['tmpc'], ['ident'])
        tsc(tmpc[:], REL[:], 0.0, None, ALU.is_ge, None, ['REL'], ['tmpc'])
        cp(TRIU[:], tmpc[:], ['tmpc'], ['TRIU'])
        tsc(tmpc[:], REL[:], 0.0, None, ALU.is_le, None, ['REL'], ['tmpc'])
        cp(TRIL[:], tmpc[:], ['tmpc'], ['TRIL'])
        memset(onesf[:], 1.0, ['onesf'])
        memset(onesb[:], 1.0, ['onesb'])
        EOFF = SB(cs, "EOFF", [128, NE])
        S.op('pool', lambda e: e.iota(EOFF[:], pattern=[[CAPT, NE]], base=0, channel_multiplier=0,
                                      allow_small_or_imprecise_dtypes=True), writes=['EOFF'])
        S.sync_all()

        def phase_end(name, l):
            if cfg.stop == name and l == cfg.dbg_layer:
                raise StopBuild()

        for l in range(L):
          try:
            src = xin if l == 0 else X
            with_ctx = (l < L - 1)
            last = (l == L - 1)

            with ExitStack() as ps:
                s2 = SB(ps, "s2", [128, KC, 2])
                wm = SB(ps, "wm", [128, KC, 512])
                bm = SB(ps, "bm", [2, 512])
                mo = SB(ps, "mo", [2, 512])
                psm = PS(ps, "psm", [2, 512])
                dma(s2[:].rearrange("p k c -> p (k c)"), cl[:, :], [], ['s2'])
                act(s2[:], s2[:], AF.Silu, ['s2'], ['s2'])
                wmv = w_mod[l].rearrange("(kc p) n -> p kc n", p=128)

                def k0_body(nb):
                    dma(wm[:], wmv[:, :, dsl(nb * 512, 512)], [], ['wm'])
                    dma(bm[:], b_mod[l:l + 1, dsl(nb * 512, 512)].partition_broadcast(2), [], ['bm'])
                    for kc in range(KC):
                        mm(psm[:], s2[:, kc, :], wm[:, kc, :], ['s2', 'wm'], ['psm'], start=(kc == 0), stop=(kc == KC - 1))
                    tt(mo[:], psm[:], bm[:], ALU.add, ['psm', 'bm'], ['mo'])
                    dma(MODV[:, dsl(nb * 512, 512)], mo[:], ['mo'], ['MODV'])
                S.loop(0, 6 * D // 512, k0_body)
                S.sync_all()

            phase_end('K0', l)
            with ExitStack() as ps:
                wf = SB(ps, "wf", [128, INW])
                wb = SB(ps, "wb", [128, INW], BF16)

                def wc_body(kc):
                    dma(wf[:], w_in[l][dsl(kc * 128, 128), :], [], ['wf'])
                    cp(wb[:, :INW // 2], wf[:, :INW // 2], ['wf'], ['wb'])
                    cp(wb[:, INW // 2:], wf[:, INW // 2:], ['wf'], ['wb'], eng='act')
                    dma(WINB[dsl(kc * 128, 128), :], wb[:], ['wb'], ['WINB'])
                S.loop(0, KC, wc_body)
                S.sync_all()

            with ExitStack() as ps:
                GMB = SB(ps, "GMB", [128, D])
                SHB = SB(ps, "SHB", [128, D])
                NGB = SB(ps, "NGB", [128, D])
                xt = SB(ps, "xt", [128, D])
                hf = SB(ps, "hf", [128, D])
                hb = SB(ps, "hb", [128, D], BF16)
                junk = SB(ps, "junk", [128, D], BF16)
                ss = SB(ps, "ss", [128, 1])
                hT = SB(ps, "hT", [128, KC, 128], BF16)
                wc = SB(ps, "wc", [128, KC, 512], BF16)
                pb = SB(ps, "pb", [128, INW], BF16)
                ptb = SB(ps, "ptb", [128, NQK, 128], BF16)
                rt = SB(ps, "rt", [128, 4 * GW])
                tA = SB(ps, "tA", [128, 512])
                tB = SB(ps, "tB", [128, 512])
                pst = PS(ps, "pst", [128, KC, 128], BF16)
                psp = PS(ps, "psp", [128, 512])
                ptp = PS(ps, "ptp", [128, NQK, 128], BF16)
                dma(NGB[:], norm1_g[l:l + 1, :].partition_broadcast(128), [], ['NGB'])
                WINBv = WINB.rearrange("(kc p) n -> p kc n", p=128)
                PTv = PT.rearrange("j p t -> p j t")

                def load_mod1(r):
                    dma(GMB[:], MODV[r:r + 1, D:2 * D].partition_broadcast(128), [], ['GMB'])
                    dma(SHB[:], MODV[r:r + 1, 0:D].partition_broadcast(128), [], ['SHB'])
                    stt(GMB[:], GMB[:], 1.0, NGB[:], ALU.add, ALU.mult, ['GMB', 'NGB'], ['GMB'])

                def k1_body(i, is_ctx):
                    r0 = i * 128
                    dma(xt[:], src[dsl(r0, 128), :], [], ['xt'])
                    if not is_ctx:
                        dma(rt[:], rope[dsl(r0, 128), :], [], ['rt'])
                    act(junk[:], xt[:], AF.Square, ['xt'], ['junk', 'ss'], accum_out=ss[:])
                    rstd_from(ss[:], D, NORM_EPS, 'ss')
                    stt(hf[:], xt[:], ss[:, 0:1], GMB[:], ALU.mult, ALU.mult, ['xt', 'ss', 'GMB'], ['hf'])
                    tt(hb[:], hf[:], SHB[:], ALU.add, ['hf', 'SHB'], ['hb'])
                    for kc in range(KC):
                        tr(pst[:, kc, :], hb[:, kc * 128:(kc + 1) * 128], ident[:], ['hb'], ['pst'])
                    half = KC // 2
                    cp(hT[:, :half, :], pst[:, :half, :], ['pst'], ['hT'])
                    cp(hT[:, half:, :], pst[:, half:, :], ['pst'], ['hT'], eng='act')
                    for (name, c0, w, rp) in cfg.chunks:
                        dma(wc[:, :, :w], WINBv[:, :, c0:c0 + w], [], ['wc'])
                        for kc in range(KC):
                            mm(psp[:, :w], hT[:, kc, :], wc[:, kc, :w], ['hT', 'wc'], ['psp'], start=(kc == 0), stop=(kc == KC - 1))
                        scale = (128 ** -0.5) if name == 'kc' else 1.0
                        if rp == 0 or is_ctx:
                            act(pb[:, c0:c0 + w], psp[:, :w], AF.Copy, ['psp'], ['pb'], scale=scale)
                        else:
                            toff = 0 if rp == 64 else 2 * GW
                            nf = rp // 4
                            cosv = rt[:, toff:toff + w]
                            sinv = rt[:, toff + GW:toff + GW + w].rearrange("p (g u f) -> p g u f", u=2, f=nf)
                            psv = psp[:, :w].rearrange("p (g u f) -> p g u f", u=2, f=nf)
                            tBv = tB[:, :w].rearrange("p (g u f) -> p g u f", u=2, f=nf)
                            tt(tA[:, :w], psp[:, :w], cosv, ALU.mult, ['psp', 'rt'], ['tA'])
                            tt(tBv[:, :, 0, :], psv[:, :, 1, :], sinv[:, :, 0, :], ALU.mult, ['psp', 'rt'], ['tB'])
                            tt(tBv[:, :, 1, :], psv[:, :, 0, :], sinv[:, :, 1, :], ALU.mult, ['psp', 'rt'], ['tB'])
                            if scale != 1.0:
                                tt(tA[:, :w], tA[:, :w], tB[:, :w], ALU.add, ['tA', 'tB'], ['tA'])
                                act(pb[:, c0:c0 + w], tA[:, :w], AF.Copy, ['tA'], ['pb'], scale=scale)
                            else:
                                tt(pb[:, c0:c0 + w], tA[:, :w], tB[:, :w], ALU.add, ['tA', 'tB'], ['pb'])
                    dma(P[dsl(r0, 128), :], pb[:], ['pb'], ['P'])
                    j = 0
                    for name in ('qa', 'ka', 'qb', 'kb', 'qc', 'kc', 'qd', 'kd'):
                        c0 = cfg.col[name]
                        nh = HKV if name == 'kd' else H
                        assert cfg.ptbase[name] == j
                        for hh in range(nh):
                            tr(ptp[:, j, :], pb[:, c0 + hh * 128:c0 + (hh + 1) * 128], ident[:], ['pb'], ['ptp'])
                            j += 1
                    hq = NQK // 2
                    cp(ptb[:, :hq, :], ptp[:, :hq, :], ['ptp'], ['ptb'])
                    cp(ptb[:, hq:, :], ptp[:, hq:, :], ['ptp'], ['ptb'], eng='act')
                    dma(PTv[:, :, dsl(r0, 128)], ptb[:], ['ptb'], ['PT'])

                load_mod1(0)
                S.loop(0, NTL, lambda i: k1_body(i, False))
                S.sync_all()
                load_mod1(1)
                for i in range(NTL, NT):
                    k1_body(i, True)
                S.sync_all()

            phase_end('K1', l)
            def attend(pool, q_ap, chunks, scale, out_sb, extra_den=None, tag=""):
                st_ps, pts, o_ps, rz = pool['st'], pool['pts'], pool['o'], pool['rz']
                n = len(chunks)
                for c, (kT, va, mk) in enumerate(chunks):
                    mm(st_ps[:, c, :], kT, q_ap, ['kq' + tag], ['st'])
                act(pts[:, :n, :], st_ps[:, :n, :], AF.Exp, ['st'], ['pts'], scale=scale)
                for c, (kT, va, mk) in enumerate(chunks):
                    if mk is not None:
                        tt(pts[:, c, :], pts[:, c, :], mk, ALU.mult, ['pts', 'mk'], ['pts'])
                for c, (kT, va, mk) in enumerate(chunks):
                    mm(o_ps[:, :], pts[:, c, :], va, ['pts', 'va' + tag], ['o'], start=(c == 0), stop=(c == n - 1))
                if extra_den is not None:
                    tt(rz[:], o_ps[:, 128:129], extra_den, ALU.add, ['o', 'sink'], ['rz'])
                    recip(rz[:], rz[:], ['rz'], ['rz'])
                else:
                    recip(rz[:], o_ps[:, 128:129], ['o'], ['rz'])
                act(out_sb, o_ps[:, 0:128], AF.Copy, ['o', 'rz'], ['osb'], scale=rz[:, 0:1])

            OTv = OT.rearrange("c p t -> p c t")
            PTv = PT.rearrange("j p t -> p j t")

            with ExitStack() as ps:
                ETAB = SB(ps, "ETAB", [128, NTYPE * H * 5 * 128], BF16)
                etf = SB(ps, "etf", [128, H * 5 * 128])
                QA = SB(ps, "QA", [128, H, 128], BF16)
                KA = SB(ps, "KA", [128, H, 5 * 128], BF16)
                VA = SB(ps, "VA", [128, 5, H, 129], BF16)
                KAc = SB(ps, "KAc", [128, H, CTX], BF16)
                VAc = SB(ps, "VAc", [128, NTC, H, 129], BF16)
                pts = SB(ps, "ptsA", [128, 5 + NTC, 128], BF16)
                rz = SB(ps, "rzA", [128, 1])
                osb = SB(ps, "osbA", [128, 128], BF16)
                oTa = SB(ps, "oTa", [128, H, 128], BF16)
                st_ps = PS(ps, "stA", [128, 8, 128])
                o_ps = PS(ps, "oA", [128, 129])
                tp_ps = PS(ps, "tpA", [128, H, 128], BF16)
                pool = {'st': st_ps, 'pts': pts, 'o': o_ps, 'rz': rz}
                tw = H * 5 * 128
                for ty in range(NTYPE):
                    dma(etf[:], nab[l, :, ty * tw:(ty + 1) * tw], [], ['etf'])
                    act(ETAB[:, ty * tw:(ty + 1) * tw], etf[:], AF.Exp, ['etf'], ['ETAB'])
                ETv = ETAB[:].rearrange("p (y h c q) -> p y h c q", y=NTYPE, h=H, c=5)
                memset(VA[:], 1.0, ['VA'])
                memset(VAc[:], 1.0, ['VAc'])
                cva = cfg.col['va']
                dma(KAc[:], PTv[:, H:2 * H, SEQ:T], [], ['KAc'])
                for c in range(NTC):
                    dma(VAc[:, c, :, 0:128], P[SEQ + c * 128:SEQ + (c + 1) * 128, cva:cva + GW].rearrange("p (h e) -> p h e", h=H), [], ['VAc'])
                S.sync_all()

                def a_body(i, ty, base, is_ctx=False):
                    r0 = i * 128
                    dma(QA[:], PTv[:, 0:H, dsl(r0, 128)], [], ['kq'])
                    if not is_ctx:
                        dma(KA[:], PTv[:, H:2 * H, dsl(base * 128, 640)], [], ['kq'])
                        for c in range(5):
                            dma(VA[:, c, :, 0:128], P[dsl((base + c) * 128, 128), cva:cva + GW].rearrange("p (h e) -> p h e", h=H), [], ['va'])
                    for h in range(H):
                        chunks = []
                        if not is_ctx:
                            for c in range(5):
                                chunks.append((KA[:, h, c * 128:(c + 1) * 128], VA[:, c, h, :], ETv[:, ty, h, c, :]))
                        for c in range(NTC):
                            chunks.append((KAc[:, h, c * 128:(c + 1) * 128], VAc[:, c, h, :], None))
                        attend(pool, QA[:, h, :], chunks, 128 ** -0.5, osb[:])
                        tr(tp_ps[:, h, :], osb[:], ident[:], ['osb'], ['tp'])
                    cp(oTa[:], tp_ps[:], ['tp'], ['oTa'])
                    dma(OTv[:, 0:H, dsl(r0, 128)], oTa[:], ['oTa'], ['OT'])

                a_body(0, 1, 0)
                a_body(1, 2, 0)
                S.loop(2, NTL - 2, lambda i: a_body(i, 0, i - 2))
                a_body(NTL - 2, 3, NTL - 5)
                a_body(NTL - 1, 4, NTL - 5)
                if with_ctx:
                    for i in range(NTL, NT):
                        a_body(i, 0, 0, is_ctx=True)
                S.sync_all()

            phase_end('A', l)
            with ExitStack() as ps:
                QD = SB(ps, "QD", [128, H, 128], BF16)
                KD = SB(ps, "KD", [128, HKV, 3 * 128], BF16)
                VD = SB(ps, "VD", [128, 3, HKV, 129], BF16)
                KDc = SB(ps, "KDc", [128, HKV, CTX], BF16)
                VDc = SB(ps, "VDc", [128, NTC, HKV, 129], BF16)
                SINKE = SB(ps, "SINKE", [128, H])
                pts = SB(ps, "ptsD", [128, 3 + NTC, 128], BF16)
                rz = SB(ps, "rzD", [128, 1])
                osb = SB(ps, "osbD", [128, 128], BF16)
                oTd = SB(ps, "oTd", [128, H, 128], BF16)
                st_ps = PS(ps, "stD", [128, 8, 128])
                o_ps = PS(ps, "oD", [128, 129])
                tp_ps = PS(ps, "tpD", [128, H, 128], BF16)
                pool = {'st': st_ps, 'pts': pts, 'o': o_ps, 'rz': rz}
                cvd = cfg.col['vd']
                jq, jk = cfg.ptbase['qd'], cfg.ptbase['kd']
                memset(VD[:], 1.0, ['VD'])
                memset(VDc[:], 1.0, ['VDc'])
                dma(KDc[:], PTv[:, jk:jk + HKV, SEQ:T], [], ['KDc'])
                for c in range(NTC):
                    dma(VDc[:, c, :, 0:128], P[SEQ + c * 128:SEQ + (c + 1) * 128, cvd:cvd + GW // 2].rearrange("p (h e) -> p h e", h=HKV), [], ['VDc'])
                dma(SINKE[:], swa_sink[l:l + 1, :].partition_broadcast(128), [], ['sink'])
                act(SINKE[:], SINKE[:], AF.Exp, ['sink'], ['sink'])
                S.sync_all()

                def d_body(i, lo, hi, is_ctx=False):
                    r0 = i * 128
                    dma(QD[:], PTv[:, jq:jq + H, dsl(r0, 128)], [], ['kq'])
                    nk = hi - lo + 1
                    if not is_ctx:
                        k0 = (i + lo) * 128
                        dma(KD[:, :, :nk * 128], PTv[:, jk:jk + HKV, dsl(k0, nk * 128)], [], ['kq'])
                        for c in range(nk):
                            dma(VD[:, c, :, 0:128], P[dsl(k0 + c * 128, 128), cvd:cvd + GW // 2].rearrange("p (h e) -> p h e", h=HKV), [], ['va'])
                    for hq in range(H):
                        g = hq // (H // HKV)
                        chunks = []
                        if not is_ctx:
                            for c in range(nk):
                                dlt = lo + c
                                mk = TRIL[:] if dlt == -1 else (TRIU[:] if dlt == 1 else None)
                                chunks.append((KD[:, g, c * 128:(c + 1) * 128], VD[:, c, g, :], mk))
                        for c in range(NTC):
                            chunks.append((KDc[:, g, c * 128:(c + 1) * 128], VDc[:, c, g, :], None))
                        attend(pool, QD[:, hq, :], chunks, 128 ** -0.5, osb[:], extra_den=SINKE[:, hq:hq + 1])
                        tr(tp_ps[:, hq, :], osb[:], ident[:], ['osb'], ['tp'])
                    cp(oTd[:], tp_ps[:], ['tp'], ['oTd'])
                    dma(OTv[:, 3 * H:4 * H, dsl(r0, 128)], oTd[:], ['oTd'], ['OT'])

                d_body(0, 0, 1)
                S.loop(1, NTL - 1, lambda i: d_body(i, -1, 1))
                d_body(NTL - 1, -1, 0)
                if with_ctx:
                    for i in range(NTL, NT):
                        d_body(i, 0, 0, is_ctx=True)
                S.sync_all()

            phase_end('D', l)
            with ExitStack() as ps:
                KTB0 = SB(ps, "KTB0", [64, T], BF16)
                KTB1 = SB(ps, "KTB1", [64, T], BF16)
                QB0 = SB(ps, "QB0", [64, 512], BF16)
                QB1 = SB(ps, "QB1", [64, 512], BF16)
                KTBm = (KTB0, KTB1)
                QBm = (QB0, QB1)
                VB = SB(ps, "VB", [128, NT, 128], BF16)
                QB = SB(ps, "QB", [128, 512], BF16)
                PTSa = SB(ps, "PTSa", [128, 2, 512], BF16)
                PTSb = SB(ps, "PTSb", [128, 2, 512], BF16)
                ZA = SB(ps, "ZA", [128, 2, 512])
                RR = SB(ps, "RR", [128, 2, 512])
                o0 = SB(ps, "o0", [128, 512])
                o1 = SB(ps, "o1", [128, 512])
                obb = SB(ps, "obb", [128, 512], BF16)
                DLB = SB(ps, "DLB", [128, 256])
                lam4 = SB(ps, "lam4", [128, 4])
                lamn = SB(ps, "lamn", [128, 1])
                DG = SB(ps, "DG", [128, 1])
                st_psa = PS(ps, "stBa", [128, 2, 512])
                st_psb = PS(ps, "stBb", [128, 2, 512])
                acc = PS(ps, "accB", [128, 2, 512])
                fin = PS(ps, "finB", [128, 2, 512])
                lam_init = 0.8 - 0.6 * math.exp(-0.3 * l)
                dma(DLB[:], diff_lambda[l:l + 1, :].partition_broadcast(128), [], ['DLB'])
                dv = DLB[:].rearrange("p (a d) -> p a d", a=4)
                tt(DLB[:, 0:64], dv[:, 0, :], dv[:, 1, :], ALU.mult, ['DLB'], ['DLB'])
                tt(DLB[:, 128:192], dv[:, 2, :], dv[:, 3, :], ALU.mult, ['DLB'], ['DLB'])
                S.op('dve', lambda e: e.tensor_reduce(out=lam4[:, 0:1], in_=DLB[:, 0:64], axis=AX.X, op=ALU.add), reads=['DLB'], writes=['lam4'])
                S.op('dve', lambda e: e.tensor_reduce(out=lam4[:, 1:2], in_=DLB[:, 128:192], axis=AX.X, op=ALU.add), reads=['DLB'], writes=['lam4'])
                act(lam4[:, 0:2], lam4[:, 0:2], AF.Exp, ['lam4'], ['lam4'])
                tt(lamn[:], lam4[:, 1:2], lam4[:, 0:1], ALU.subtract, ['lam4'], ['lamn'])
                tsc(lamn[:], lamn[:], -lam_init, None, ALU.add, None, ['lamn'], ['lamn'])
                dma(DG[:], diff_norm_g[l:l + 1, :].rearrange("o e -> e o"), [], ['DG'])
                tsc(DG[:], DG[:], 1.0 - lam_init, None, ALU.mult, None, ['DG'], ['DG'])
                cvb = cfg.col['vb']
                KG = 5

                def b_block(h, q0, nq, kchunks):
                    dma(QB0[:, :nq], PT[2 * H + h][0:64, dsl(q0, nq)], [], ['QB'])
                    dma(QB1[:, :nq], PT[2 * H + h][64:128, dsl(q0, nq)], [], ['QB'])
                    memset(ZA[:], 0.0, ['ZA'])

                    def step(kc, first, lastf, par=0):
                        st_ps = st_psa if par == 0 else st_psb
                        PTS = PTSa if par == 0 else PTSb
                        ks, kp = 'st%d' % par, 'PTS%d' % par
                        for m in range(2):
                            mm(st_ps[:, m, :nq], KTBm[m][:, dsl(kc * 128, 128)], QBm[m][:, :nq],
                               ['KTB', 'QB'], [ks])
                        act(PTS[:, :, :nq], st_ps[:, :, :nq], AF.Exp, [ks], [kp], scale=64 ** -0.5)
                        tt(ZA[:, :, :nq], ZA[:, :, :nq], PTS[:, :, :nq], ALU.add, ['ZA', kp], ['ZA'])
                        for m in range(2):
                            mm(acc[:, m, :nq], VB[:, dsl(kc, 1), :].rearrange("p o e -> p (o e)"), PTS[:, m, :nq], ['VB', kp], ['acc'],
                               start=first, stop=lastf)
                    n = len(kchunks)
                    ng = (n + KG - 1) // KG
                    if ng <= 2:
                        for j, kc in enumerate(kchunks):
                            step(kc, j == 0, j == n - 1, j % 2)
                    else:
                        assert n % KG == 0 and kchunks == list(range(n))
                        for j in range(KG):
                            step(j, j == 0, False, j % 2)

                        def mid(gidx):
                            for j in range(KG):
                                step(gidx * KG + j, False, False, j % 2)
                        S.loop(1, ng - 1, mid)
                        for j in range(KG):
                            kc = (ng - 1) * KG + j
                            step(kc, False, j == KG - 1, j % 2)
                    for m in range(2):
                        mm(fin[:, m, :nq], onesf[:], ZA[:, m, :nq], ['ZA'], ['fin'])
                    S.op('dve', lambda e: e.reciprocal(out=RR[:, :, :nq], in_=fin[:, :, :nq]), reads=['fin'], writes=['RR'])
                    tt(o0[:, :nq], acc[:, 0, :nq], RR[:, 0, :nq], ALU.mult, ['acc', 'RR'], ['o0'])
                    tt(o1[:, :nq], acc[:, 1, :nq], RR[:, 1, :nq], ALU.mult, ['acc', 'RR'], ['o1'])
                    stt(o0[:, :nq], o1[:, :nq], lamn[:, 0:1], o0[:, :nq], ALU.mult, ALU.add, ['o0', 'o1', 'lamn'], ['o0'])
                    tt(o1[:, :nq], o0[:, :nq], o0[:, :nq], ALU.mult, ['o0'], ['o1'])
                    mm(fin[:, 0, :nq], onesf[:], o1[:, :nq], ['o1'], ['fin'])
                    tsc(o1[:, :nq], fin[:, 0, :nq], 1.0 / 128, NORM_EPS, ALU.mult, ALU.add, ['fin'], ['o1'])
                    act(o1[:, :nq], o1[:, :nq], AF.Sqrt, ['o1'], ['o1'])
                    S.op('dve', lambda e: e.reciprocal(out=o1[:, :nq], in_=o1[:, :nq]), reads=['o1'], writes=['o1'])
                    tt(o0[:, :nq], o0[:, :nq], o1[:, :nq], ALU.mult, ['o0', 'o1'], ['o0'])
                    act(obb[:, :nq], o0[:, :nq], AF.Copy, ['o0', 'DG'], ['obb'], scale=DG[:, 0:1])
                    dma(OT[H + h][:, dsl(q0, nq)], obb[:, :nq], ['obb'], ['OT'])

                QBW = min(512, SEQ)
                for h in range(H):
                    S.sync_all()
                    dma(KTB0[:], PT[3 * H + h][0:64, :], [], ['KTB'])
                    dma(KTB1[:], PT[3 * H + h][64:128, :], [], ['KTB'])
                    dma(VB[:], P[:, cvb + h * 128:cvb + (h + 1) * 128].rearrange("(n p) e -> p n e", p=128), [], ['VB'])
                    S.sync_all()
                    nqb = SEQ // QBW
                    if nqb == 1:
                        b_block(h, 0, QBW, list(range(NT)))
                    else:
                        S.sync_all()
                        with S.fori(0, nqb) as qi:
                            b_block(h, qi * QBW, QBW, list(range(NT)))
                            S.sync_all()
                    if with_ctx:
                        b_block(h, SEQ, CTX, list(range(NTL, NT)))
                S.sync_all()

            phase_end('B', l)
            with ExitStack() as ps:
                DEC = SB(ps, "DEC", [128, 2, H])
                DT = SB(ps, "DT", [128, 2, H, 128], BF16)
                QDEC = SB(ps, "QDEC", [128, 2, H])
                KDEC = SB(ps, "KDEC", [128, 2, H])
                CDEC = SB(ps, "CDEC", [128, 2, H])
                PIDX = SB(ps, "PIDX", [128, 1])
                tmpd = SB(ps, "tmpd", [128, 128])
                mskf = SB(ps, "mskf", [128, 128])
                Sst = SB(ps, "Sst", [128, H, 128])
                Sb = SB(ps, "Sb", [128, H, 128], BF16)
                QT = SB(ps, "QTc", [128, H, 128], BF16)
                KT = SB(ps, "KTc", [128, H, 128], BF16)
                Kt = SB(ps, "Ktok", [128, GW], BF16)
                Vt = SB(ps, "Vtok", [128, GW], BF16)
                Gt = SB(ps, "Gtok", [128, GW], BF16)
                Kd = SB(ps, "Kd", [128, 128], BF16)
                ATm = SB(ps, "ATm", [128, 128], BF16)
                tmpo = SB(ps, "tmpo", [128, 128])
                of = SB(ps, "of", [128, GW])
                oft = SB(ps, "oft", [128, GW])
                sg = SB(ps, "sg", [128, GW])
                GNG = SB(ps, "GNG", [128, GW])
                GNB = SB(ps, "GNB", [128, GW])
                st1 = SB(ps, "st1", [128, 1])
                st2 = SB(ps, "st2", [128, 1])
                yob = SB(ps, "yob", [128, GW], BF16)
                oTc = SB(ps, "oTc", [128, H, 128], BF16)
                at_ps = PS(ps, "atC", [128, 128])
                o1_ps = PS(ps, "o1C", [128, 128])
                o2_ps = PS(ps, "o2C", [128, 128])
                m_ps = PS(ps, "mC", [128, 128])
                tp_ps = PS(ps, "tpC", [128, H, 128], BF16)
                dma(DEC[:, 0, :], ret_decay_fwd[l:l + 1, :].partition_broadcast(128), [], ['DEC'])
                dma(DEC[:, 1, :], ret_decay_bwd[l:l + 1, :].partition_broadcast(128), [], ['DEC'])
                dma(GNG[:], ret_gn_g[l:l + 1, :].partition_broadcast(128), [], ['GNG'])
                dma(GNB[:], ret_gn_b[l:l + 1, :].partition_broadcast(128), [], ['GNB'])
                act(DEC[:], DEC[:], AF.Exp, ['DEC'], ['DEC'], scale=-1.0)
                act(DEC[:], DEC[:], AF.Ln, ['DEC'], ['DEC'], bias=1.0)
                tsc(DEC[:], DEC[:], -1.0, None, ALU.mult, None, ['DEC'], ['DEC'])
                tsc(PIDX[:], REL[:, 0:1], -1.0, None, ALU.mult, None, [], ['PIDX'])
                for d in range(2):
                    sgn = 1.0 if d == 0 else -1.0
                    tsc(tmpd[:], REL[:], sgn, 0.0, ALU.mult, ALU.max, [], ['tmpd'])
                    tsc(mskf[:], REL[:], sgn, 0.0, ALU.mult, ALU.is_ge, [], ['mskf'])
                    for h in range(H):
                        act(tmpo[:], tmpd[:], AF.Exp, ['tmpd', 'DEC'], ['tmpo'], scale=DEC[:, d, h:h + 1])
                        tt(DT[:, d, h, :], tmpo[:], mskf[:], ALU.mult, ['tmpo', 'mskf'], ['DT'])
                        if d == 0:
                            tsc(st1[:], PIDX[:], 1.0, None, ALU.add, None, ['PIDX'], ['st1'])
                        else:
                            tsc(st1[:], PIDX[:], -1.0, 128.0, ALU.mult, ALU.add, ['PIDX'], ['st1'])
                        act(QDEC[:, d, h:h + 1], st1[:], AF.Exp, ['st1', 'DEC'], ['QDEC'], scale=DEC[:, d, h:h + 1])
                        if d == 0:
                            tsc(st1[:], PIDX[:], -1.0, 127.0, ALU.mult, ALU.add, ['PIDX'], ['st1'])
                        else:
                            tsc(st1[:], PIDX[:], 1.0, None, ALU.mult, None, ['PIDX'], ['st1'])
                        act(KDEC[:, d, h:h + 1], st1[:], AF.Exp, ['st1', 'DEC'], ['KDEC'], scale=DEC[:, d, h:h + 1])
                act(CDEC[:], DEC[:], AF.Exp, ['DEC'], ['CDEC'], scale=128.0)
                cqc, ckc, cvc, cgc = cfg.col['qc'], cfg.col['kc'], cfg.col['vc'], cfg.col['gc']
                jq, jk = cfg.ptbase['qc'], cfg.ptbase['kc']
                S.sync_all()

                def c_body(i, d):
                    r0 = i * 128
                    dma(QT[:], PTv[:, jq:jq + H, dsl(r0, 128)], [], ['QT'])
                    dma(KT[:], PTv[:, jk:jk + H, dsl(r0, 128)], [], ['KT'])
                    dma(Kt[:], P[dsl(r0, 128), ckc:ckc + GW], [], ['Kt'])
                    dma(Vt[:], P[dsl(r0, 128), cvc:cvc + GW], [], ['Vt'])
                    if d == 1:
                        dma(Gt[:], P[dsl(r0, 128), cgc:cgc + GW], [], ['Gt'])
                        dma(oft[:], OF[dsl(r0, 128), :], [], ['oft'])
                    for h in range(H):
                        hs = slice(h * 128, (h + 1) * 128)
                        mm(at_ps[:], KT[:, h, :], QT[:, h, :], ['KT', 'QT'], ['at'])
                        tt(ATm[:], at_ps[:], DT[:, d, h, :], ALU.mult, ['at'], ['ATm'])
                        mm(o1_ps[:], ATm[:], Vt[:, hs], ['ATm', 'Vt'], ['o1p'])
                        mm(o2_ps[:], QT[:, h, :], Sb[:, h, :], ['QT', 'Sb'], ['o2p'])
                        act(tmpo[:], o2_ps[:], AF.Copy, ['o2p'], ['tmpo'], scale=QDEC[:, d, h:h + 1])
                        tt(of[:, hs], o1_ps[:], tmpo[:], ALU.add, ['o1p', 'tmpo'], ['of'])
                        tsc(Kd[:], Kt[:, hs], KDEC[:, d, h:h + 1], None, ALU.mult, None, ['Kt'], ['Kd'])
                        mm(m_ps[:], Kd[:], Vt[:, hs], ['Kd', 'Vt'], ['mp'])
                        stt(Sst[:, h, :], Sst[:, h, :], CDEC[:, d, h:h + 1], m_ps[:], ALU.mult, ALU.add, ['Sst', 'mp'], ['Sst'])
                        cp(Sb[:, h, :], Sst[:, h, :], ['Sst'], ['Sb'], eng='act')
                    if d == 0:
                        dma(OF[dsl(r0, 128), :], of[:], ['of'], ['OF'])
                    else:
                        tt(of[:], of[:], oft[:], ALU.add, ['of', 'oft'], ['of'])
                        for h in range(H):
                            hs = slice(h * 128, (h + 1) * 128)
                            S.op('dve', lambda e: e.tensor_reduce(out=st1[:], in_=of[:, hs], axis=AX.X, op=ALU.add), reads=['of'], writes=['st1'])
                            tsc(st1[:], st1[:], -1.0 / 128, None, ALU.mult, None, ['st1'], ['st1'])
                            tsc(of[:, hs], of[:, hs], st1[:, 0:1], None, ALU.add, None, ['of', 'st1'], ['of'])
                            act(tmpo[:], of[:, hs], AF.Square, ['of'], ['tmpo', 'st2'], accum_out=st2[:])
                            rstd_from(st2[:], 128, GN_EPS, 'st2')
                            tsc(of[:, hs], of[:, hs], st2[:, 0:1], None, ALU.mult, None, ['of', 'st2'], ['of'])
                        tt(of[:], of[:], GNG[:], ALU.mult, ['of'], ['of'])
                        tt(of[:], of[:], GNB[:], ALU.add, ['of'], ['of'])
                        act(sg[:], Gt[:], AF.Silu, ['Gt'], ['sg'])
                        tt(yob[:], of[:], sg[:], ALU.mult, ['of', 'sg'], ['yob'])
                        for h in range(H):
                            tr(tp_ps[:, h, :], yob[:, h * 128:(h + 1) * 128], ident[:], ['yob'], ['tp'])
                        cp(oTc[:], tp_ps[:], ['tp'], ['oTc'])
                        dma(OTv[:, 2 * H:3 * H, dsl(r0, 128)], oTc[:], ['oTc'], ['OT'])

                for d in range(2):
                    memset(Sst[:], 0.0, ['Sst'])
                    memset(Sb[:], 0.0, ['Sb'])
                    ctx_order = list(range(NTL, NT)) if d == 0 else list(range(NT - 1, NTL - 1, -1))
                    for i in ctx_order:
                        c_body(i, d)
                    if d == 0:
                        S.loop(0, NTL, lambda i: c_body(i, 0))
                    else:
                        S.loop(0, NTL, lambda i: c_body(NTL - 1 - i, 1))
                    S.sync_all()

            phase_end('C', l)
            with ExitStack() as ps:
                WOB = SB(ps, "WOB", [128, 4 * H, D], BF16)
                wof = SB(ps, "wof", [128, D])
                G1B = SB(ps, "G1B", [128, D])
                GM2B = SB(ps, "GM2B", [128, D])
                SH2B = SB(ps, "SH2B", [128, D])
                N2B = SB(ps, "N2B", [128, D])
                xt = SB(ps, "xtF", [128, D])
                x1 = SB(ps, "x1F", [128, D])
                h2f = SB(ps, "h2f", [128, D])
                h2b = SB(ps, "h2b", [128, D], BF16)
                junk = SB(ps, "junkF", [128, D], BF16)
                oT = SB(ps, "oTF", [128, 4 * H, 128], BF16)
                h2T = SB(ps, "h2T", [128, KC, 128], BF16)
                WRB = SB(ps, "WRB", [128, KC, NE], BF16)
                wrf = SB(ps, "wrf", [128, KC, NE])
                ss = SB(ps, "ssF", [128, 1])
                lg = SB(ps, "lgF", [128, NE])
                mx = SB(ps, "mxF", [128, 1])
                sm = SB(ps, "smF", [128, 1])
                affs = SB(ps, "affs", [128, NE])
                po = PS(ps, "poF", [128, 512])
                pst = PS(ps, "pstF", [128, KC, 128], BF16)
                pr = PS(ps, "prF", [128, NE])
                for c in range(4 * H):
                    dma(wof[:], w_out[l][c * 128:(c + 1) * 128, :], [], ['wof'])
                    cp(WOB[:, c, :], wof[:], ['wof'], ['WOB'], eng=('dve' if c % 2 == 0 else 'act'))
                dma(wrf[:], w_router[l].rearrange("(kc p) e -> p kc e", p=128), [], ['wrf'])
                cp(WRB[:], wrf[:], ['wrf'], ['WRB'])
                dma(N2B[:], norm2_g[l:l + 1, :].partition_broadcast(128), [], ['N2B'])

                def load_mod2(r):
                    dma(G1B[:], MODV[r:r + 1, 2 * D:3 * D].partition_broadcast(128), [], ['G1B'])
                    dma(GM2B[:], MODV[r:r + 1, 4 * D:5 * D].partition_broadcast(128), [], ['GM2B'])
                    dma(SH2B[:], MODV[r:r + 1, 3 * D:4 * D].partition_broadcast(128), [], ['SH2B'])
                    stt(GM2B[:], GM2B[:], 1.0, N2B[:], ALU.add, ALU.mult, ['GM2B', 'N2B'], ['GM2B'])

                def f_body(i):
                    r0 = i * 128
                    dma(oT[:], OTv[:, :, dsl(r0, 128)], [], ['oT'])
                    dma(xt[:], src[dsl(r0, 128), :], [], ['xt'])
                    for n in range(ND):
                        ns = slice(n * NW, (n + 1) * NW)
                        for c in range(4 * H):
                            mm(po[:, :NW], oT[:, c, :], WOB[:, c, ns], ['oT', 'WOB'], ['po'], start=(c == 0), stop=(c == 4 * H - 1))
                        tt(x1[:, ns], po[:, :NW], G1B[:, ns], ALU.mult, ['po', 'G1B'], ['x1'])
                    tt(x1[:], x1[:], xt[:], ALU.add, ['x1', 'xt'], ['x1'])
                    dma(X[dsl(r0, 128), :], x1[:], ['x1'], ['X'])
                    act(junk[:], x1[:], AF.Square, ['x1'], ['junk', 'ss'], accum_out=ss[:])
                    rstd_from(ss[:], D, NORM_EPS, 'ss')
                    stt(h2f[:], x1[:], ss[:, 0:1], GM2B[:], ALU.mult, ALU.mult, ['x1', 'ss', 'GM2B'], ['h2f'])
                    tt(h2b[:], h2f[:], SH2B[:], ALU.add, ['h2f', 'SH2B'], ['h2b'])
                    dma(H2[dsl(r0, 128), :], h2b[:], ['h2b'], ['H2'])
                    for kc in range(KC):
                        tr(pst[:, kc, :], h2b[:, kc * 128:(kc + 1) * 128], ident[:], ['h2b'], ['pst'])
                    cp(h2T[:], pst[:], ['pst'], ['h2T'])
                    for kc in range(KC):
                        mm(pr[:], h2T[:, kc, :], WRB[:, kc, :], ['h2T', 'WRB'], ['pr'], start=(kc == 0), stop=(kc == KC - 1))
                    cp(lg[:], pr[:], ['pr'], ['lg'])
                    S.op('dve', lambda e: e.tensor_reduce(out=mx[:], in_=lg[:], axis=AX.X, op=ALU.max), reads=['lg'], writes=['mx'])
                    tsc(mx[:], mx[:], -1.0, None, ALU.mult, None, ['mx'], ['mx'])
                    act(affs[:], lg[:], AF.Exp, ['lg', 'mx'], ['affs', 'sm'], bias=mx[:, 0:1], accum_out=sm[:])
                    recip(sm[:], sm[:], ['sm'], ['sm'])
                    tsc(AFFT[:, dsl(i, 1), :].rearrange("p o e -> p (o e)"), affs[:], sm[:, 0:1], None, ALU.mult, None, ['affs', 'sm'], ['AFFT'])

                load_mod2(0)
                S.loop(0, NTL, f_body)
                S.sync_all()
                if with_ctx:
                    load_mod2(1)
                    for i in range(NTL, NT):
                        f_body(i)
                    S.sync_all()

            phase_end('F', l)
            groups = [(0, NTL, CAP)] + ([(NTL, NT, CAPC)] if with_ctx else [])
            with ExitStack() as ps:
                THR = SB(ps, "THR", [128, 2, NE])
                HI = SB(ps, "HI", [128, 2, NE])
                MID = SB(ps, "MID", [128, 2, NE])
                cmpt = SB(ps, "cmpt", [128, NTL])
                cnt = SB(ps, "cnt", [128, 2, NE])
                gef = SB(ps, "gef", [128, 2, NE])
                dlt = SB(ps, "dlt", [128, 2, NE])
                OFF = SB(ps, "OFF", [128, NE])
                mk = SB(ps, "mkM", [128, NE])
                mkb = SB(ps, "mkbM", [128, NE], BF16)
                t1 = SB(ps, "t1M", [128, NE])
                posc = SB(ps, "posc", [128, NE], I32)
                idx1 = SB(ps, "idx1", [128, 1], I32)
                h2t = SB(ps, "h2tM", [128, D], BF16)
                tot = PS(ps, "totM", [128, 2, NE])
                cum = PS(ps, "cumM", [128, NE])
                tot2 = PS(ps, "tot2M", [128, NE])
                memset(THR[:], 0.0, ['THR'])
                memset(HI[:], 1.0, ['HI'])
                memset(MID[:], 0.5, ['MID'])
                memset(cnt[:], 0.0, ['cnt'])
                S.sync_all()

                def bis_body(it):
                    for gi, (t0, t1_, cap) in enumerate(groups):
                        for e in range(NE):
                            tsc(cmpt[:, :t1_ - t0], AFFT[:, t0:t1_, e], MID[:, gi, e:e + 1], None, ALU.is_ge, None, ['MID'], ['cmpt'])
                            S.op('dve', lambda en: en.tensor_reduce(out=cnt[:, gi, e:e + 1], in_=cmpt[:, :t1_ - t0], axis=AX.X, op=ALU.add),
                                 reads=['cmpt'], writes=['cnt'])
                    mm(tot[:].rearrange("p a e -> p (a e)"), onesf[:], cnt[:].rearrange("p a e -> p (a e)"), ['cnt'], ['tot'])
                    for gi, (t0, t1_, cap) in enumerate(groups):
                        tsc(gef[:, gi, :], tot[:, gi, :], float(cap) - 0.5, None, ALU.is_ge, None, ['tot'], ['gef'])
                    if len(groups) == 1:
                        memset(gef[:, 1, :], 0.0, ['gef'])
                    tt(dlt[:], MID[:], THR[:], ALU.subtract, ['MID', 'THR'], ['dlt'])
                    tt(dlt[:], dlt[:], gef[:], ALU.mult, ['dlt', 'gef'], ['dlt'])
                    tt(THR[:], THR[:], dlt[:], ALU.add, ['THR', 'dlt'], ['THR'])
                    tt(dlt[:], HI[:], MID[:], ALU.subtract, ['HI', 'MID'], ['dlt'])
                    tt(dlt[:], dlt[:], gef[:], ALU.mult, ['dlt', 'gef'], ['dlt'])
                    tt(HI[:], MID[:], dlt[:], ALU.add, ['MID', 'dlt'], ['HI'])
                    tt(MID[:], THR[:], HI[:], ALU.add, ['THR', 'HI'], ['MID'])
                    tsc(MID[:], MID[:], 0.5, None, ALU.mult, None, ['MID'], ['MID'])
                S.loop(0, 40, bis_body)
                S.sync_all()
                memset(OFF[:], 0.0, ['OFF'])
                BIG = float(NE * CAPT + 64)

                def m2_body(i, gi):
                    r0 = i * 128
                    a = AFFT[:, dsl(i, 1), :].rearrange("p o e -> p (o e)")
                    tt(mk[:], a, THR[:, gi, :], ALU.is_ge, ['AFFT', 'THR'], ['mk'])
                    cp(mkb[:], mk[:], ['mk'], ['mkb'])
                    mm(cum[:], TRIU[:], mkb[:], ['mkb'], ['cum'])
                    mm(tot2[:], onesb[:], mkb[:], ['mkb'], ['tot2'])
                    tt(t1[:], cum[:], OFF[:], ALU.add, ['cum', 'OFF'], ['t1'])
                    tt(t1[:], t1[:], EOFF[:], ALU.add, ['t1'], ['t1'])
                    tsc(t1[:], t1[:], -1.0 - BIG, None, ALU.add, None, ['t1'], ['t1'])
                    tt(t1[:], t1[:], mk[:], ALU.mult, ['t1', 'mk'], ['t1'])
                    tsc(t1[:], t1[:], BIG, None, ALU.add, None, ['t1'], ['t1'])
                    cp(posc[:], t1[:], ['t1'], ['posc'])
                    cp(POSI[:, dsl(i, 1), :].rearrange("p o e -> p (o e)"), posc[:], ['posc'], ['POSI'])
                    tt(WGT[:, dsl(i, 1), :].rearrange("p o e -> p (o e)"), a, mk[:], ALU.mult, ['AFFT', 'mk'], ['WGT'])
                    tt(OFF[:], OFF[:], tot2[:], ALU.add, ['OFF', 'tot2'], ['OFF'])
                    dma(h2t[:], H2[dsl(r0, 128), :], [], ['h2t'])
                    for e in range(NE if not getattr(cfg, 'no_scatter', False) else 0):
                        cp(idx1[:], posc[:, e:e + 1], ['posc'], ['idx1'])
                        ind_dma(lambda en: en.indirect_dma_start(
                            out=XEf[:, :], out_offset=bass.IndirectOffsetOnAxis(ap=idx1[:, :], axis=0),
                            in_=h2t[:, :], in_offset=None, bounds_check=NE * CAPT - 1, oob_is_err=False),
                            NE * CAPT - 1, ['h2t', 'idx1'], ['XE'])

                for i in range(NTL):
                    m2_body(i, 0)
                    if i % 16 == 15:
                        S.sync_all()
                S.sync_all()
                if with_ctx:
                    for i in range(NTL, NT):
                        m2_body(i, 1)
                    S.sync_all()

            phase_end('M2', l)
            with ExitStack() as ps:
                WG = SB(ps, "WG", [128, KC, FF], BF16)
                WU = SB(ps, "WU", [128, KC, FF], BF16)
                WD = SB(ps, "WD", [128, FC, D], BF16)
                stg = SB(ps, "stg", [128, 4096])
                xe = SB(ps, "xe", [128, D], BF16)
                xT = SB(ps, "xT", [128, KC, 512], BF16)
                hT = SB(ps, "hTM", [128, FC, 512], BF16)
                sgt = SB(ps, "sgt", [128, 512])
                yt = SB(ps, "ytM", [128, D], BF16)
                pst = PS(ps, "pstM", [128, KC, 128], BF16)
                pg = PS(ps, "pgM", [128, 512])
                pu = PS(ps, "puM", [128, 512])
                pd = PS(ps, "pdM", [128, 512])
                blocks = [(s0, min(512, CAP - s0)) for s0 in range(0, CAP, 512)]
                if with_ctx:
                    blocks += [(CAP + s0, min(512, CAPC - s0)) for s0 in range(0, CAPC, 512)]
                kstep = max(1, 4096 // FF)
                fstep = max(1, 4096 // D)
                wgl, wul, wdl = w_gate[l], w_up[l], w_down[l]
                cnt_eng = [0]

                def conv(out, in_, R, W):
                    eng = ('dve', 'act', 'pool')[cnt_eng[0] % 3]
                    cnt_eng[0] += 1
                    cp(out, in_, R, W, eng=eng)

                def m3_body(e):
                    for (wsrc, wdst) in ((wgl, WG), (wul, WU)):
                        wv = wsrc[dsl(e, 1)].rearrange("o (kc p) f -> p (o kc) f", p=128)
                        for k0 in range(0, KC, kstep):
                            dma(stg[:, :kstep * FF].rearrange("p (k f) -> p k f", f=FF), wv[:, k0:k0 + kstep, :], [], ['stg'])
                            conv(wdst[:, k0:k0 + kstep, :], stg[:, :kstep * FF].rearrange("p (k f) -> p k f", f=FF), ['stg'], ['W'])
                    wv = wdl[dsl(e, 1)].rearrange("o (fc p) n -> p (o fc) n", p=128)
                    for f0 in range(0, FC, fstep):
                        dma(stg[:, :fstep * D].rearrange("p (k f) -> p k f", f=D), wv[:, f0:f0 + fstep, :], [], ['stg'])
                        conv(WD[:, f0:f0 + fstep, :], stg[:, :fstep * D].rearrange("p (k f) -> p k f", f=D), ['stg'], ['W'])
                    XEe = XE[dsl(e, 1)].rearrange("o s d -> (o s) d")
                    YEe = YE[dsl(e, 1)].rearrange("o s d -> (o s) d")
                    for (s0, nb) in blocks:
                        for st0 in range(0, nb, 128):
                            ns_ = min(128, nb - st0)
                            dma(xe[:ns_, :], XEe[s0 + st0:s0 + st0 + ns_, :], [], ['xe'])
                            for kc in range(KC):
                                tr(pst[:, kc, :ns_], xe[:ns_, kc * 128:(kc + 1) * 128], ident[:ns_, :ns_], ['xe'], ['pst'])
                            cp(xT[:, :, st0:st0 + ns_], pst[:, :, :ns_], ['pst'], ['xT'])
                        for fc in range(FC):
                            for kc in range(KC):
                                mm(pg[:, :nb], WG[:, kc, fc * 128:(fc + 1) * 128], xT[:, kc, :nb], ['W', 'xT'], ['pg'], start=(kc == 0), stop=(kc == KC - 1))
                            for kc in range(KC):
                                mm(pu[:, :nb], WU[:, kc, fc * 128:(fc + 1) * 128], xT[:, kc, :nb], ['W', 'xT'], ['pu'], start=(kc == 0), stop=(kc == KC - 1))
                            act(sgt[:, :nb], pg[:, :nb], AF.Silu, ['pg'], ['sgt'])
                            tt(hT[:, fc, :nb], sgt[:, :nb], pu[:, :nb], ALU.mult, ['sgt', 'pu'], ['hTM'])
                        for st0 in range(0, nb, 128):
                            ns_ = min(128, nb - st0)
                            for n in range(ND):
                                nsl = slice(n * NW, (n + 1) * NW)
                                for fc in range(FC):
                                    mm(pd[:ns_, :NW], hT[:, fc, st0:st0 + ns_], WD[:, fc, nsl], ['hTM', 'W'], ['pd'], start=(fc == 0), stop=(fc == FC - 1))
                                cp(yt[:ns_, nsl], pd[:ns_, :NW], ['pd'], ['yt'], eng=('act' if n % 2 else 'dve'))
                            dma(YEe[s0 + st0:s0 + st0 + ns_, :], yt[:ns_, :], ['yt'], ['YE'])
                S.loop(0, NE, m3_body)
                S.sync_all()

            phase_end('M3', l)
            with ExitStack() as ps:
                G2B = SB(ps, "G2B", [128, D])
                FNB = SB(ps, "FNB", [128, D])
                x1 = SB(ps, "x1C", [128, D])
                accm = SB(ps, "accm", [128, D])
                Gg = SB(ps, "Gg", [128, D], BF16)
                posc = SB(ps, "poscC", [128, NE], I32)
                idx1c = SB(ps, "idx1c", [128, 1], I32)
                wcc = SB(ps, "wcc", [128, NE])
                junk = SB(ps, "junkC", [128, D], BF16)
                ss = SB(ps, "ssC", [128, 1])
                memset(Gg[:], 0.0, ['Gg'])
                if last:
                    dma(FNB[:], final_norm_g[0:1, :].partition_broadcast(128), [], ['FNB'])

                def m4_body(i):
                    r0 = i * 128
                    dma(x1[:], X[dsl(r0, 128), :], ['X'], ['x1'])
                    cp(posc[:], POSI[:, dsl(i, 1), :].rearrange("p o e -> p (o e)"), ['POSI'], ['posc'])
                    cp(wcc[:], WGT[:, dsl(i, 1), :].rearrange("p o e -> p (o e)"), ['WGT'], ['wcc'])
                    memset(accm[:], 0.0, ['accm'])
                    for e in range(NE):
                        cp(idx1c[:], posc[:, e:e + 1], ['posc'], ['idx1c'])
                        ind_dma(lambda en: en.indirect_dma_start(
                            out=Gg[:, :], out_offset=None, in_=YEf[:, :],
                            in_offset=bass.IndirectOffsetOnAxis(ap=idx1c[:, :], axis=0),
                            bounds_check=NE * CAPT - 1, oob_is_err=False), NE * CAPT - 1, ['idx1c', 'YE', 'Gg'], ['Gg'])
                        stt(accm[:], Gg[:], wcc[:, e:e + 1], accm[:], ALU.mult, ALU.add, ['Gg', 'wcc', 'accm'], ['accm'])
                    tt(accm[:], accm[:], G2B[:], ALU.mult, ['accm', 'G2B'], ['accm'])
                    tt(x1[:], x1[:], accm[:], ALU.add, ['x1', 'accm'], ['x1'])
                    if not last:
                        dma(X[dsl(r0, 128), :], x1[:], ['x1'], ['X'])
                    else:
                        act(junk[:], x1[:], AF.Square, ['x1'], ['junk', 'ss'], accum_out=ss[:])
                        rstd_from(ss[:], D, NORM_EPS, 'ss')
                        stt(accm[:], x1[:], ss[:, 0:1], FNB[:], ALU.mult, ALU.mult, ['x1', 'ss', 'FNB'], ['accm'])
                        dma(yout[dsl(r0, 128), :], accm[:], ['accm'], ['yout'])

                dma(G2B[:], MODV[0:1, 5 * D:6 * D].partition_broadcast(128), [], ['G2B'])
                for i in range(NTL):
                    m4_body(i)
                    if i % 16 == 15:
                        S.sync_all()
                S.sync_all()
                if with_ctx:
                    dma(G2B[:], MODV[1:2, 5 * D:6 * D].partition_broadcast(128), [], ['G2B'])
                    for i in range(NTL, NT):
                        m4_body(i)
                    S.sync_all()

            phase_end('M4', l)
          except StopBuild:
            S.sync_all()
            for nm, (src_ap, dst_ap) in dbg.items():
                dma(dst_ap, src_ap, [], ['dbg' + nm])
            S.sync_all()
            break
        S.sync_all()
        print("instructions:", S.ninst)
    return nc


def rope_tables(cfg):
    SEQ, GW = cfg.SEQ, cfg.GW
    t = np.arange(SEQ)
    row = (t // GRID_W).astype(np.float32)
    col = (t % GRID_W).astype(np.float32)
    out = np.zeros((SEQ, 4 * GW), np.float32)
    for k, dim in enumerate((64, 128)):
        nf = dim // 4
        inv = (ROPE_BASE ** (-np.arange(nf, dtype=np.float32) / nf)).astype(np.float32)
        ar = (row[:, None] * inv).astype(np.float32)
        ac = (col[:, None] * inv).astype(np.float32)
        cosh = np.concatenate([np.cos(ar), np.cos(ar), np.cos(ac), np.cos(ac)], axis=1)
        sinh = np.concatenate([-np.sin(ar), np.sin(ar), -np.sin(ac), np.sin(ac)], axis=1)
        nh = GW // dim
        out[:, (2 * k) * GW:(2 * k + 1) * GW] = np.tile(cosh, (1, nh))
        out[:, (2 * k + 1) * GW:(2 * k + 2) * GW] = np.tile(sinh, (1, nh))
    return out.astype(np.float32)


def na_bias_tables(cfg, rpb):
    L, H, NTL = cfg.L, cfg.H, cfg.NTL
    rows = NTL * 2
    cols = np.arange(GRID_W)
    cstart = np.clip(cols - 8, 0, GRID_W - 16)
    kc = cols[:, None]
    qc = cols[None, :]
    ok = (kc >= cstart[None, :]) & (kc < cstart[None, :] + 16)
    co = np.clip(kc - qc + 15, 0, 30)
    out = np.full((L, cfg.NTYPE, H, 128, 5, 128), NEG_INF, np.float32)
    types = [2, 0, 1, NTL - 2, NTL - 1]
    for ti, i in enumerate(types):
        base = min(max(i - 2, 0), NTL - 5)
        for c in range(5):
            kt = base + c
            for kr2 in range(2):
                krow = 2 * kt + kr2
                for qr2 in range(2):
                    r = 2 * i + qr2
                    st = min(max(r - 4, 0), rows - 8)
                    if st <= krow < st + 8:
                        ro = krow - r + 7
                        vals = rpb[:, :, ro, :][:, :, co]
                        vals = np.where(ok[None, None], vals, np.float32(NEG_INF))
                        out[:, ti, :, kr2 * 64:(kr2 + 1) * 64, c, qr2 * 64:(qr2 + 1) * 64] = vals
    out = out.transpose(0, 3, 1, 2, 4, 5).reshape(L, 128, cfg.NTYPE * H * 5 * 128)
    return np.ascontiguousarray(out)


def make_in_maps(cfg, inp):
    B = inp['x'].shape[0]
    rope = rope_tables(cfg)
    nab = na_bias_tables(cfg, np.asarray(inp['na_rpb'], np.float32))
    maps = []
    for b in range(B):
        cl = np.stack([np.asarray(inp['c'][b]).reshape(cfg.KC, 128).T, np.asarray(inp['c_ctx']).reshape(cfg.KC, 128).T], axis=-1)
        m = {
            'xin': np.ascontiguousarray(np.concatenate([inp['x'][b], inp['ctx'][b]], axis=0)),
            'cl': np.ascontiguousarray(cl.reshape(128, cfg.KC * 2)).astype(np.float32),
            'nab': nab, 'rope': rope,
            'diff_lambda': np.asarray(inp['diff_lambda']).reshape(cfg.L, 256),
            'final_norm_g': np.asarray(inp['final_norm_g']).reshape(1, cfg.D),
        }
        for k in ('w_mod', 'b_mod', 'norm1_g', 'w_in', 'diff_norm_g', 'ret_decay_fwd', 'ret_decay_bwd', 'ret_gn_g',
                  'ret_gn_b', 'swa_sink', 'w_out', 'norm2_g', 'w_router', 'w_gate', 'w_up', 'w_down'):
            m[k] = np.asarray(inp[k], np.float32)
        maps.append(m)
    return maps


def kernel(**inputs):
    cfg = Cfg()
    nc = build_program(cfg)
    maps = make_in_maps(cfg, inputs)
    res = run_bass_kernel_spmd(nc, maps, core_ids=list(range(len(maps))))
    return np.stack([np.asarray(r['y']) for r in res.results], axis=0).astype(np.float32)
```

```python
import math
import numpy as np
from contextlib import ExitStack
import concourse.bass as bass
import concourse.mybir as mybir
from concourse.bass_utils import run_bass_kernel_spmd

F32 = mybir.dt.float32
BF16 = mybir.dt.bfloat16
I32 = mybir.dt.int32
AF = mybir.ActivationFunctionType
ALU = mybir.AluOpType
AX = mybir.AxisListType

NORM_EPS = 1e-6
GN_EPS = 1e-5
NEG_INF = -1e30
ROPE_BASE = 10000.0
GRID_W = 64


def dsl(v, n):
    if isinstance(v, (int, np.integer)):
        return slice(int(v), int(v) + n)
    return bass.ds(v, n)


class StopBuild(Exception):
    pass


class Cfg:
    def __init__(self, D=2048, SEQ=16384, CTX=256, NE=16, L=2):
        self.D, self.SEQ, self.CTX, self.NE, self.L = D, SEQ, CTX, NE, L
        self.GW = D // 4
        self.H = self.GW // 128
        self.HKV = self.H // 2
        self.FF = D // 2
        self.KC = D // 128
        self.FC = self.FF // 128
        self.INW = 11 * self.GW + 2 * (self.GW // 2)
        self.T = SEQ + CTX
        self.NTL = SEQ // 128
        self.NTC = CTX // 128
        self.NT = self.NTL + self.NTC
        self.CAP = 2 * SEQ // NE
        self.CAPC = 2 * CTX // NE
        self.CAPT = self.CAP + self.CAPC
        GW = self.GW
        names = ['qa', 'ka', 'va', 'qb', 'kb', 'vb', 'qc', 'kc', 'vc', 'gc', 'qd', 'kd', 'vd']
        widths = [GW] * 11 + [GW // 2] * 2
        ropes = [0, 0, 0, 64, 64, 0, 128, 128, 0, 0, 128, 128, 0]
        self.chunks = []
        c0 = 0
        for n, w, r in zip(names, widths, ropes):
            self.chunks.append((n, c0, w, r))
            c0 += w
        self.col = {n: c for (n, c, w, r) in self.chunks}
        H = self.H
        self.ptbase = {'qa': 0, 'ka': H, 'qb': 2 * H, 'kb': 3 * H, 'qc': 4 * H, 'kc': 5 * H, 'qd': 6 * H, 'kd': 7 * H}
        self.NQK = 7 * H + self.HKV
        self.NTYPE = 5
        self.stop = None
        self.dbg_layer = 0


class Sched:
    NS_DMA = 8

    def __init__(self, nc, es):
        self.nc = nc
        self.es = es
        self.engs = {'pe': nc.tensor, 'act': nc.scalar, 'dve': nc.vector, 'pool': nc.gpsimd, 'sp': nc.sync}
        self.csem = {}
        self.ccnt = {}
        self.semobj = {}
        for e in ('pe', 'act', 'dve', 'pool'):
            n = f"c_{e}"
            s = es.enter_context(nc.semaphore(n))
            self.csem[e] = (n, s)
            self.semobj[n] = s
            self.ccnt[e] = 0
        self.dsem = {}
        self.dcnt = {}
        for q in ('sp', 'pool', 'act'):
            self.dsem[q] = []
            for i in range(self.NS_DMA):
                n = f"d_{q}_{i}"
                s = es.enter_context(nc.semaphore(n))
                self.semobj[n] = s
                self.dsem[q].append(n)
            self.dcnt[q] = 0
        self.known = {e: {} for e in self.engs}
        self.res = {}
        self.ninst = 0

    def _wait(self, e, ev):
        if ev is None:
            return
        name, val = ev
        k = self.known[e]
        if k.get(name, 0) >= val:
            return
        self.engs[e].wait_ge(self.semobj[name], val)
        self.ninst += 1
        k[name] = val

    def _deps(self, e, reads, writes, same_ok=False):
        evs = []
        for r in reads:
            st = self.res.get(r)
            if st and st['w']:
                evs.append(st['w'])
        for w in writes:
            st = self.res.get(w)
            if st:
                if st['w']:
                    evs.append(st['w'])
                evs.extend(st['r'])
        best = {}
        for (n, v, src) in evs:
            if same_ok and src == e:
                continue
            if best.get(n, 0) < v:
                best[n] = v
        for n, v in best.items():
            self._wait(e, (n, v))

    def _record(self, e, ev, reads, writes):
        n, v = ev
        rec = (n, v, e)
        for r in reads:
            st = self.res.setdefault(r, {'w': None, 'r': []})
            st['r'].append(rec)
            if len(st['r']) > 48:
                best = {}
                for (nn, vv, ss) in st['r']:
                    if best.get(nn, (0, None))[0] < vv:
                        best[nn] = (vv, ss)
                st['r'] = [(nn, vv, ss) for nn, (vv, ss) in best.items()]
        for w in writes:
            self.res[w] = {'w': rec, 'r': []}

    def op(self, e, fn, reads=(), writes=(), same_ok=False):
        self._deps(e, reads, writes, same_ok=same_ok)
        inst = fn(self.engs[e])
        name, sem = self.csem[e]
        self.ccnt[e] += 1
        inst.then_inc(sem, 1)
        self._record(e, (name, self.ccnt[e]), reads, writes)
        self.ninst += 1
        return inst

    def dma(self, q, fn, reads=(), writes=()):
        self._deps(q, reads, writes)
        i = self.dcnt[q]
        self.dcnt[q] += 1
        name = self.dsem[q][i % self.NS_DMA]
        val = 16 * (i // self.NS_DMA + 1)
        if val > 16:
            self._wait(q, (name, val - 16))
        inst = fn(self.engs[q])
        inst.then_inc(self.semobj[name], 16)
        self._record(q, (name, val), reads, writes)
        self.ninst += 1
        return inst

    def sync_all(self):
        nc = self.nc
        evs = []
        for e, (name, sem) in self.csem.items():
            if self.ccnt[e] > 0:
                evs.append((name, self.ccnt[e]))
        for q, names in self.dsem.items():
            cnt = self.dcnt[q]
            for j, name in enumerate(names):
                k = (cnt - j + self.NS_DMA - 1) // self.NS_DMA
                if k > 0:
                    evs.append((name, 16 * k))
        for ev in evs:
            self._wait('sp', ev)
        for (name, val) in evs:
            if name.startswith('d_pool_'):
                continue
            nc.sync.sem_clear(self.semobj[name])
        nc.all_engine_barrier()
        self.ninst += 2 + len(self.semobj)
        for e in self.ccnt:
            self.ccnt[e] = 0
        for q in self.dcnt:
            if q != 'pool':
                self.dcnt[q] = 0
        self.known = {e: {} for e in self.engs}
        self.res = {}

    def loop(self, start, end, body):
        if end <= start:
            return
        if end - start == 1:
            body(start)
            return
        self.sync_all()
        with self.fori(start, end) as i:
            body(i)
            self.sync_all()

    def fori(self, start, end):
        from contextlib import contextmanager
        nc = self.nc
        if not hasattr(self, 'loop_regs'):
            self.loop_regs = []
            self.loop_depth = 0

        if not hasattr(self, 'reg_pfx'):
            self.reg_pfx = []
            self.freed = set()
            for en in self.engs.values():
                r = en.alloc_register("mkprobe")
                self.reg_pfx.append((en, r.name[:-len("mkprobe")], r.engine))
                en.free_register(r)

        @contextmanager
        def _loop():
            d = self.loop_depth
            id0 = nc.next_id()
            while len(self.loop_regs) <= d:
                self.loop_regs.append(nc.alloc_registers(f"mkloop{len(self.loop_regs)}", engines=mybir.ALL_ENGINES))
            registers = self.loop_regs[d]
            self.loop_depth += 1
            lid = nc.next_id()
            loop_start = f"mk_fori_{lid}_loop"
            loop_end = f"mk_fori_{lid}_end"
            nc.regs_mov(registers, start)
            nc.br(loop_start, engines=mybir.ALL_ENGINES)
            with nc.body(loop_start, valid_engines=mybir.ALL_ENGINES):
                yield nc.snap(registers, min_val=start, max_val=end - 1)
                nc.regs_alu(registers, registers, 1, op=mybir.AluOpType.add)
                nc.br_lt(registers, end, on_true=loop_start, on_false=loop_end, engines=mybir.ALL_ENGINES)
            nc.switch_bb(loop_end)
            self.loop_depth -= 1
            id1 = nc.next_id()
            for (en, pfx, et) in self.reg_pfx:
                for k in range(id0, id1):
                    for nm in (f"{pfx}tmp_{k}", f"{pfx}{pfx}mkloop{d}_snap_{k}"):
                        if nm in self.freed:
                            continue
                        try:
                            en.free_register(bass.RegisterHandle(nm, et))
                            self.freed.add(nm)
                        except Exception:
                            pass
        return _loop()


def build_program(cfg, debug=False):
    D, SEQ, CTX, NE, L = cfg.D, cfg.SEQ, cfg.CTX, cfg.NE, cfg.L
    GW, H, HKV, FF, KC, FC, INW = cfg.GW, cfg.H, cfg.HKV, cfg.FF, cfg.KC, cfg.FC, cfg.INW
    T, NTL, NTC, NT = cfg.T, cfg.NTL, cfg.NTC, cfg.NT
    CAP, CAPC, CAPT = cfg.CAP, cfg.CAPC, cfg.CAPT
    NQK, NTYPE = cfg.NQK, cfg.NTYPE
    ND = D // 512 if D >= 512 else 1
    NW = min(512, D)
    nc = bass.Bass("TRN2", target_bir_lowering=False)

    def din(name, shape, dt=F32):
        return nc.dram_tensor(name, list(shape), dt, kind="ExternalInput").ap()

    def dscr(name, shape, dt):
        return nc.dram_tensor(name, list(shape), dt, kind="Internal").ap()

    xin = din("xin", [T, D])
    cl = din("cl", [128, KC * 2])
    w_mod = din("w_mod", [L, D, 6 * D])
    b_mod = din("b_mod", [L, 6 * D])
    norm1_g = din("norm1_g", [L, D])
    w_in = din("w_in", [L, D, INW])
    nab = din("nab", [L, 128, NTYPE * H * 5 * 128])
    diff_lambda = din("diff_lambda", [L, 256])
    diff_norm_g = din("diff_norm_g", [L, 128])
    ret_decay_fwd = din("ret_decay_fwd", [L, H])
    ret_decay_bwd = din("ret_decay_bwd", [L, H])
    ret_gn_g = din("ret_gn_g", [L, GW])
    ret_gn_b = din("ret_gn_b", [L, GW])
    swa_sink = din("swa_sink", [L, H])
    w_out = din("w_out", [L, D, D])
    norm2_g = din("norm2_g", [L, D])
    w_router = din("w_router", [L, D, NE])
    w_gate = din("w_gate", [L, NE, D, FF])
    w_up = din("w_up", [L, NE, D, FF])
    w_down = din("w_down", [L, NE, FF, D])
    final_norm_g = din("final_norm_g", [1, D])
    rope = din("rope", [SEQ, 4 * GW])
    yout = nc.dram_tensor("y", [SEQ, D], F32, kind="ExternalOutput").ap()

    X = dscr("X", [T, D], F32)
    MODV = dscr("MODV", [2, 6 * D], F32)
    WINB = dscr("WINB", [D, INW], BF16)
    P = dscr("P", [T, INW], BF16)
    PT = dscr("PT", [NQK, 128, T], BF16)
    OT = dscr("OT", [4 * H, 128, T], BF16)
    OF = dscr("OF", [T, GW], F32)
    H2 = dscr("H2", [T, D], BF16)
    XEf = dscr("XE", [NE * CAPT, D], BF16)
    YEf = dscr("YE", [NE * CAPT, D], BF16)
    XE = XEf.rearrange("(e s) d -> e s d", e=NE)
    YE = YEf.rearrange("(e s) d -> e s d", e=NE)
    dbg = {}
    if debug:
        for nm, ap_, dt in (("P", P, BF16), ("PT", PT, BF16), ("OT", OT, BF16), ("X", X, F32), ("MODV", MODV, F32), ("H2", H2, BF16)):
            dbg[nm] = (ap_, nc.dram_tensor("dbg_" + nm, list(ap_.shape), dt, kind="ExternalOutput").ap())

    with ExitStack() as es:
        S = Sched(nc, es)

        def dma(out, in_, R, W, q='sp'):
            S.dma(q, lambda e: e.dma_start(out=out, in_=in_), reads=R, writes=W)

        ind_state = []

        def ind_dma(fn, bc, R, W):
            id0 = nc.next_id()
            S.dma('pool', fn, reads=R, writes=W)
            id1 = nc.next_id()
            if not ind_state:
                r = nc.gpsimd.alloc_register("mkp")
                ind_state.append((r.name[:-3], r.engine))
                nc.gpsimd.free_register(r)
            pfx, et = ind_state[0]
            for k in range(id0, id1 + 1):
                try:
                    nc.gpsimd.free_register(bass.RegisterHandle(f"{pfx}val_{bc}_{k}", et))
                except Exception:
                    pass

        def mm(out, lhsT, rhs, R, W, start=True, stop=True):
            S.op('pe', lambda e: e.matmul(out, lhsT=lhsT, rhs=rhs, start=start, stop=stop), reads=R, writes=W, same_ok=True)

        def tr(out, in_, ident, R, W):
            S.op('pe', lambda e: e.transpose(out=out, in_=in_, identity=ident), reads=R, writes=W, same_ok=True)

        def act(out, in_, func, R, W, **kw):
            S.op('act', lambda e: e.activation(out=out, in_=in_, func=func, **kw), reads=R, writes=W)

        def tt(out, a, b, op, R, W, eng='dve'):
            S.op(eng, lambda e: e.tensor_tensor(out=out, in0=a, in1=b, op=op), reads=R, writes=W)

        def tsc(out, a, s1, s2, op0, op1, R, W, eng='dve'):
            if s2 is None:
                S.op(eng, lambda e: e.tensor_scalar(out=out, in0=a, scalar1=s1, scalar2=None, op0=op0), reads=R, writes=W)
            else:
                S.op(eng, lambda e: e.tensor_scalar(out=out, in0=a, scalar1=s1, scalar2=s2, op0=op0, op1=op1), reads=R, writes=W)

        def stt(out, a, s, b, op0, op1, R, W, eng='dve'):
            S.op(eng, lambda e: e.scalar_tensor_tensor(out=out, in0=a, scalar=s, in1=b, op0=op0, op1=op1), reads=R, writes=W)

        def cp(out, in_, R, W, eng='dve'):
            if eng == 'act':
                act(out, in_, AF.Copy, R, W)
            else:
                S.op(eng, lambda e: e.tensor_copy(out=out, in_=in_), reads=R, writes=W)

        def memset(ap_, val, W, eng='dve'):
            S.op(eng, lambda e: e.memset(ap_, val), writes=W)

        def recip(out, in_, R, W):
            S.op('dve', lambda e: e.reciprocal(out=out, in_=in_), reads=R, writes=W)

        def rstd_from(ss, n, eps, tag):
            tsc(ss, ss, 1.0 / n, eps, ALU.mult, ALU.add, [tag], [tag])
            act(ss, ss, AF.Sqrt, [tag], [tag])
            recip(ss, ss, [tag], [tag])

        cs = ExitStack()
        es.enter_context(cs)

        uid = [0]

        def SB(stack, name, shape, dt=F32):
            uid[0] += 1
            return stack.enter_context(nc.sbuf_tensor(f"{name}_{uid[0]}", list(shape), dt))

        def PS(stack, name, shape, dt=F32):
            uid[0] += 1
            return stack.enter_context(nc.psum_tensor(f"{name}_{uid[0]}", list(shape), dt))

        REL = SB(cs, "REL", [128, 128])
        ident = SB(cs, "ident", [128, 128], BF16)
        onesf = SB(cs, "onesf", [128, 128])
        onesb = SB(cs, "onesb", [128, 128], BF16)
        TRIU = SB(cs, "TRIU", [128, 128], BF16)
        TRIL = SB(cs, "TRIL", [128, 128], BF16)
        tmpc = SB(cs, "tmpc", [128, 128])
        AFFT = SB(cs, "AFFT", [128, NT, NE])
        POSI = SB(cs, "POSI", [128, NT, NE], I32)
        WGT = SB(cs, "WGT", [128, NT, NE])
        S.op('pool', lambda e: e.iota(REL[:], pattern=[[1, 128]], base=0, channel_multiplier=-1,
                                      allow_small_or_imprecise_dtypes=True), writes=['REL'])
        tsc(tmpc[:], REL[:], 0.0, None, ALU.is_equal, None, ['REL'], ['tmpc'])
        cp(ident[:], tmpc[:], ['tmpc'], ['ident'])
        tsc(tmpc[:], REL[:], 0.0, None, ALU.is_ge, None, ['REL'], ['tmpc'])
        cp(TRIU[:], tmpc[:], ['tmpc'], ['TRIU'])
        tsc(tmpc[:], REL[:], 0.0, None, ALU.is_le, None, ['REL'], ['tmpc'])
        cp(TRIL[:], tmpc[:], ['tmpc'], ['TRIL'])
        memset(onesf[:], 1.0, ['onesf'])
        memset(onesb[:], 1.0, ['onesb'])
        EOFF = SB(cs, "EOFF", [128, NE])
        S.op('pool', lambda e: e.iota(EOFF[:], pattern=[[CAPT, NE]], base=0, channel_multiplier=0,
                                      allow_small_or_imprecise_dtypes=True), writes=['EOFF'])
        S.sync_all()

        def phase_end(name, l):
            if cfg.stop == name and l == cfg.dbg_layer:
                raise StopBuild()

        for l in range(L):
          try:
            src = xin if l == 0 else X
            with_ctx = (l < L - 1)
            last = (l == L - 1)

            with ExitStack() as ps:
                s2 = SB(ps, "s2", [128, KC, 2])
                wm = SB(ps, "wm", [128, KC, 512])
                bm = SB(ps, "bm", [2, 512])
                mo = SB(ps, "mo", [2, 512])
                psm = PS(ps, "psm", [2, 512])
                dma(s2[:].rearrange("p k c -> p (k c)"), cl[:, :], [], ['s2'])
                act(s2[:], s2[:], AF.Silu, ['s2'], ['s2'])
                wmv = w_mod[l].rearrange("(kc p) n -> p kc n", p=128)

                def k0_body(nb):
                    dma(wm[:], wmv[:, :, dsl(nb * 512, 512)], [], ['wm'])
                    dma(bm[:], b_mod[l:l + 1, dsl(nb * 512, 512)].partition_broadcast(2), [], ['bm'])
                    for kc in range(KC):
                        mm(psm[:], s2[:, kc, :], wm[:, kc, :], ['s2', 'wm'], ['psm'], start=(kc == 0), stop=(kc == KC - 1))
                    tt(mo[:], psm[:], bm[:], ALU.add, ['psm', 'bm'], ['mo'])
                    dma(MODV[:, dsl(nb * 512, 512)], mo[:], ['mo'], ['MODV'])
                S.loop(0, 6 * D // 512, k0_body)
                S.sync_all()

            phase_end('K0', l)
            with ExitStack() as ps:
                wf = SB(ps, "wf", [128, INW])
                wb = SB(ps, "wb", [128, INW], BF16)

                def wc_body(kc):
                    dma(wf[:], w_in[l][dsl(kc * 128, 128), :], [], ['wf'])
                    cp(wb[:, :INW // 2], wf[:, :INW // 2], ['wf'], ['wb'])
                    cp(wb[:, INW // 2:], wf[:, INW // 2:], ['wf'], ['wb'], eng='act')
                    dma(WINB[dsl(kc * 128, 128), :], wb[:], ['wb'], ['WINB'])
                S.loop(0, KC, wc_body)
                S.sync_all()

            with ExitStack() as ps:
                GMB = SB(ps, "GMB", [128, D])
                SHB = SB(ps, "SHB", [128, D])
                NGB = SB(ps, "NGB", [128, D])
                xt = SB(ps, "xt", [128, D])
                hf = SB(ps, "hf", [128, D])
                hb = SB(ps, "hb", [128, D], BF16)
                junk = SB(ps, "junk", [128, D], BF16)
                ss = SB(ps, "ss", [128, 1])
                hT = SB(ps, "hT", [128, KC, 128], BF16)
                wc = SB(ps, "wc", [128, KC, 512], BF16)
                pb = SB(ps, "pb", [128, INW], BF16)
                ptb = SB(ps, "ptb", [128, NQK, 128], BF16)
                rt = SB(ps, "rt", [128, 4 * GW])
                tA = SB(ps, "tA", [128, 512])
                tB = SB(ps, "tB", [128, 512])
                pst = PS(ps, "pst", [128, KC, 128], BF16)
                psp = PS(ps, "psp", [128, 512])
                ptp = PS(ps, "ptp", [128, NQK, 128], BF16)
                dma(NGB[:], norm1_g[l:l + 1, :].partition_broadcast(128), [], ['NGB'])
                WINBv = WINB.rearrange("(kc p) n -> p kc n", p=128)
                PTv = PT.rearrange("j p t -> p j t")

                def load_mod1(r):
                    dma(GMB[:], MODV[r:r + 1, D:2 * D].partition_broadcast(128), [], ['GMB'])
                    dma(SHB[:], MODV[r:r + 1, 0:D].partition_broadcast(128), [], ['SHB'])
                    stt(GMB[:], GMB[:], 1.0, NGB[:], ALU.add, ALU.mult, ['GMB', 'NGB'], ['GMB'])

                def k1_body(i, is_ctx):
                    r0 = i * 128
                    dma(xt[:], src[dsl(r0, 128), :], [], ['xt'])
                    if not is_ctx:
                        dma(rt[:], rope[dsl(r0, 128), :], [], ['rt'])
                    act(junk[:], xt[:], AF.Square, ['xt'], ['junk', 'ss'], accum_out=ss[:])
                    rstd_from(ss[:], D, NORM_EPS, 'ss')
                    stt(hf[:], xt[:], ss[:, 0:1], GMB[:], ALU.mult, ALU.mult, ['xt', 'ss', 'GMB'], ['hf'])
                    tt(hb[:], hf[:], SHB[:], ALU.add, ['hf', 'SHB'], ['hb'])
                    for kc in range(KC):
                        tr(pst[:, kc, :], hb[:, kc * 128:(kc + 1) * 128], ident[:], ['hb'], ['pst'])
                    half = KC // 2
                    cp(hT[:, :half, :], pst[:, :half, :], ['pst'], ['hT'])
                    cp(hT[:, half:, :], pst[:, half:, :], ['pst'], ['hT'], eng='act')
                    for (name, c0, w, rp) in cfg.chunks:
                        dma(wc[:, :, :w], WINBv[:, :, c0:c0 + w], [], ['wc'])
                        for kc in range(KC):
                            mm(psp[:, :w], hT[:, kc, :], wc[:, kc, :w], ['hT', 'wc'], ['psp'], start=(kc == 0), stop=(kc == KC - 1))
                        scale = (128 ** -0.5) if name == 'kc' else 1.0
                        if rp == 0 or is_ctx:
                            act(pb[:, c0:c0 + w], psp[:, :w], AF.Copy, ['psp'], ['pb'], scale=scale)
                        else:
                            toff = 0 if rp == 64 else 2 * GW
                            nf = rp // 4
                            cosv = rt[:, toff:toff + w]
                            sinv = rt[:, toff + GW:toff + GW + w].rearrange("p (g u f) -> p g u f", u=2, f=nf)
                            psv = psp[:, :w].rearrange("p (g u f) -> p g u f", u=2, f=nf)
                            tBv = tB[:, :w].rearrange("p (g u f) -> p g u f", u=2, f=nf)
                            tt(tA[:, :w], psp[:, :w], cosv, ALU.mult, ['psp', 'rt'], ['tA'])
                            tt(tBv[:, :, 0, :], psv[:, :, 1, :], sinv[:, :, 0, :], ALU.mult, ['psp', 'rt'], ['tB'])
                            tt(tBv[:, :, 1, :], psv[:, :, 0, :], sinv[:, :, 1, :], ALU.mult, ['psp', 'rt'], ['tB'])
                            if scale != 1.0:
                                tt(tA[:, :w], tA[:, :w], tB[:, :w], ALU.add, ['tA', 'tB'], ['tA'])
                                act(pb[:, c0:c0 + w], tA[:, :w], AF.Copy, ['tA'], ['pb'], scale=scale)
                            else:
                                tt(pb[:, c0:c0 + w], tA[:, :w], tB[:, :w], ALU.add, ['tA', 'tB'], ['pb'])
                    dma(P[dsl(r0, 128), :], pb[:], ['pb'], ['P'])
                    j = 0
                    for name in ('qa', 'ka', 'qb', 'kb', 'qc', 'kc', 'qd', 'kd'):
                        c0 = cfg.col[name]
                        nh = HKV if name == 'kd' else H
                        assert cfg.ptbase[name] == j
                        for hh in range(nh):
                            tr(ptp[:, j, :], pb[:, c0 + hh * 128:c0 + (hh + 1) * 128], ident[:], ['pb'], ['ptp'])
                            j += 1
                    hq = NQK // 2
                    cp(ptb[:, :hq, :], ptp[:, :hq, :], ['ptp'], ['ptb'])
                    cp(ptb[:, hq:, :], ptp[:, hq:, :], ['ptp'], ['ptb'], eng='act')
                    dma(PTv[:, :, dsl(r0, 128)], ptb[:], ['ptb'], ['PT'])

                load_mod1(0)
                S.loop(0, NTL, lambda i: k1_body(i, False))
                S.sync_all()
                load_mod1(1)
                for i in range(NTL, NT):
                    k1_body(i, True)
                S.sync_all()

            phase_end('K1', l)
            def attend(pool, q_ap, chunks, scale, out_sb, extra_den=None, tag=""):
                st_ps, pts, o_ps, rz = pool['st'], pool['pts'], pool['o'], pool['rz']
                n = len(chunks)
                for c, (kT, va, mk) in enumerate(chunks):
                    mm(st_ps[:, c, :], kT, q_ap, ['kq' + tag], ['st'])
                act(pts[:, :n, :], st_ps[:, :n, :], AF.Exp, ['st'], ['pts'], scale=scale)
                for c, (kT, va, mk) in enumerate(chunks):
                    if mk is not None:
                        tt(pts[:, c, :], pts[:, c, :], mk, ALU.mult, ['pts', 'mk'], ['pts'])
                for c, (kT, va, mk) in enumerate(chunks):
                    mm(o_ps[:, :], pts[:, c, :], va, ['pts', 'va' + tag], ['o'], start=(c == 0), stop=(c == n - 1))
                if extra_den is not None:
                    tt(rz[:], o_ps[:, 128:129], extra_den, ALU.add, ['o', 'sink'], ['rz'])
                    recip(rz[:], rz[:], ['rz'], ['rz'])
                else:
                    recip(rz[:], o_ps[:, 128:129], ['o'], ['rz'])
                act(out_sb, o_ps[:, 0:128], AF.Copy, ['o', 'rz'], ['osb'], scale=rz[:, 0:1])

            OTv = OT.rearrange("c p t -> p c t")
            PTv = PT.rearrange("j p t -> p j t")

            with ExitStack() as ps:
                ETAB = SB(ps, "ETAB", [128, NTYPE * H * 5 * 128], BF16)
                etf = SB(ps, "etf", [128, H * 5 * 128])
                QA = SB(ps, "QA", [128, H, 128], BF16)
                KA = SB(ps, "KA", [128, H, 5 * 128], BF16)
                VA = SB(ps, "VA", [128, 5, H, 129], BF16)
                KAc = SB(ps, "KAc", [128, H, CTX], BF16)
                VAc = SB(ps, "VAc", [128, NTC, H, 129], BF16)
                pts = SB(ps, "ptsA", [128, 5 + NTC, 128], BF16)
                rz = SB(ps, "rzA", [128, 1])
                osb = SB(ps, "osbA", [128, 128], BF16)
                oTa = SB(ps, "oTa", [128, H, 128], BF16)
                st_ps = PS(ps, "stA", [128, 8, 128])
                o_ps = PS(ps, "oA", [128, 129])
                tp_ps = PS(ps, "tpA", [128, H, 128], BF16)
                pool = {'st': st_ps, 'pts': pts, 'o': o_ps, 'rz': rz}
                tw = H * 5 * 128
                for ty in range(NTYPE):
                    dma(etf[:], nab[l, :, ty * tw:(ty + 1) * tw], [], ['etf'])
                    act(ETAB[:, ty * tw:(ty + 1) * tw], etf[:], AF.Exp, ['etf'], ['ETAB'])
                ETv = ETAB[:].rearrange("p (y h c q) -> p y h c q", y=NTYPE, h=H, c=5)
                memset(VA[:], 1.0, ['VA'])
                memset(VAc[:], 1.0, ['VAc'])
                cva = cfg.col['va']
                dma(KAc[:], PTv[:, H:2 * H, SEQ:T], [], ['KAc'])
                for c in range(NTC):
                    dma(VAc[:, c, :, 0:128], P[SEQ + c * 128:SEQ + (c + 1) * 128, cva:cva + GW].rearrange("p (h e) -> p h e", h=H), [], ['VAc'])
                S.sync_all()

                def a_body(i, ty, base, is_ctx=False):
                    r0 = i * 128
                    dma(QA[:], PTv[:, 0:H, dsl(r0, 128)], [], ['kq'])
                    if not is_ctx:
                        dma(KA[:], PTv[:, H:2 * H, dsl(base * 128, 640)], [], ['kq'])
                        for c in range(5):
                            dma(VA[:, c, :, 0:128], P[dsl((base + c) * 128, 128), cva:cva + GW].rearrange("p (h e) -> p h e", h=H), [], ['va'])
                    for h in range(H):
                        chunks = []
                        if not is_ctx:
                            for c in range(5):
                                chunks.append((KA[:, h, c * 128:(c + 1) * 128], VA[:, c, h, :], ETv[:, ty, h, c, :]))
                        for c in range(NTC):
                            chunks.append((KAc[:, h, c * 128:(c + 1) * 128], VAc[:, c, h, :], None))
                        attend(pool, QA[:, h, :], chunks, 128 ** -0.5, osb[:])
                        tr(tp_ps[:, h, :], osb[:], ident[:], ['osb'], ['tp'])
                    cp(oTa[:], tp_ps[:], ['tp'], ['oTa'])
                    dma(OTv[:, 0:H, dsl(r0, 128)], oTa[:], ['oTa'], ['OT'])

                a_body(0, 1, 0)
                a_body(1, 2, 0)
                S.loop(2, NTL - 2, lambda i: a_body(i, 0, i - 2))
                a_body(NTL - 2, 3, NTL - 5)
                a_body(NTL - 1, 4, NTL - 5)
                if with_ctx:
                    for i in range(NTL, NT):
                        a_body(i, 0, 0, is_ctx=True)
                S.sync_all()

            phase_end('A', l)
            with ExitStack() as ps:
                QD = SB(ps, "QD", [128, H, 128], BF16)
                KD = SB(ps, "KD", [128, HKV, 3 * 128], BF16)
                VD = SB(ps, "VD", [128, 3, HKV, 129], BF16)
                KDc = SB(ps, "KDc", [128, HKV, CTX], BF16)
                VDc = SB(ps, "VDc", [128, NTC, HKV, 129], BF16)
                SINKE = SB(ps, "SINKE", [128, H])
                pts = SB(ps, "ptsD", [128, 3 + NTC, 128], BF16)
                rz = SB(ps, "rzD", [128, 1])
                osb = SB(ps, "osbD", [128, 128], BF16)
                oTd = SB(ps, "oTd", [128, H, 128], BF16)
                st_ps = PS(ps, "stD", [128, 8, 128])
                o_ps = PS(ps, "oD", [128, 129])
                tp_ps = PS(ps, "tpD", [128, H, 128], BF16)
                pool = {'st': st_ps, 'pts': pts, 'o': o_ps, 'rz': rz}
                cvd = cfg.col['vd']
                jq, jk = cfg.ptbase['qd'], cfg.ptbase['kd']
                memset(VD[:], 1.0, ['VD'])
                memset(VDc[:], 1.0, ['VDc'])
                dma(KDc[:], PTv[:, jk:jk + HKV, SEQ:T], [], ['KDc'])
                for c in range(NTC):
                    dma(VDc[:, c, :, 0:128], P[SEQ + c * 128:SEQ + (c + 1) * 128, cvd:cvd + GW // 2].rearrange("p (h e) -> p h e", h=HKV), [], ['VDc'])
                dma(SINKE[:], swa_sink[l:l + 1, :].partition_broadcast(128), [], ['sink'])
                act(SINKE[:], SINKE[:], AF.Exp, ['sink'], ['sink'])
                S.sync_all()

                def d_body(i, lo, hi, is_ctx=False):
                    r0 = i * 128
                    dma(QD[:], PTv[:, jq:jq + H, dsl(r0, 128)], [], ['kq'])
                    nk = hi - lo + 1
                    if not is_ctx:
                        k0 = (i + lo) * 128
                        dma(KD[:, :, :nk * 128], PTv[:, jk:jk + HKV, dsl(k0, nk * 128)], [], ['kq'])
                        for c in range(nk):
                            dma(VD[:, c, :, 0:128], P[dsl(k0 + c * 128, 128), cvd:cvd + GW // 2].rearrange("p (h e) -> p h e", h=HKV), [], ['va'])
                    for hq in range(H):
                        g = hq // (H // HKV)
                        chunks = []
                        if not is_ctx:
                            for c in range(nk):
                                dlt = lo + c
                                mk = TRIL[:] if dlt == -1 else (TRIU[:] if dlt == 1 else None)
                                chunks.append((KD[:, g, c * 128:(c + 1) * 128], VD[:, c, g, :], mk))
                        for c in range(NTC):
                            chunks.append((KDc[:, g, c * 128:(c + 1) * 128], VDc[:, c, g, :], None))
                        attend(pool, QD[:, hq, :], chunks, 128 ** -0.5, osb[:], extra_den=SINKE[:, hq:hq + 1])
                        tr(tp_ps[:, hq, :], osb[:], ident[:], ['osb'], ['tp'])
                    cp(oTd[:], tp_ps[:], ['tp'], ['oTd'])
                    dma(OTv[:, 3 * H:4 * H, dsl(r0, 128)], oTd[:], ['oTd'], ['OT'])

                d_body(0, 0, 1)
                S.loop(1, NTL - 1, lambda i: d_body(i, -1, 1))
                d_body(NTL - 1, -1, 0)
                if with_ctx:
                    for i in range(NTL, NT):
                        d_body(i, 0, 0, is_ctx=True)
                S.sync_all()

            phase_end('D', l)
            with ExitStack() as ps:
                KTB0 = SB(ps, "KTB0", [64, T], BF16)
                KTB1 = SB(ps, "KTB1", [64, T], BF16)
                QB0 = SB(ps, "QB0", [64, 512], BF16)
                QB1 = SB(ps, "QB1", [64, 512], BF16)
                KTBm = (KTB0, KTB1)
                QBm = (QB0, QB1)
                VB = SB(ps, "VB", [128, NT, 128], BF16)
                QB = SB(ps, "QB", [128, 512], BF16)
                PTS = SB(ps, "PTS", [128, 2, 512], BF16)
                ZA = SB(ps, "ZA", [128, 2, 512])
                RR = SB(ps, "RR", [128, 2, 512])
                o0 = SB(ps, "o0", [128, 512])
                o1 = SB(ps, "o1", [128, 512])
                obb = SB(ps, "obb", [128, 512], BF16)
                DLB = SB(ps, "DLB", [128, 256])
                lam4 = SB(ps, "lam4", [128, 4])
                lamn = SB(ps, "lamn", [128, 1])
                DG = SB(ps, "DG", [128, 1])
                st_ps = PS(ps, "stB", [128, 2, 512])
                acc = PS(ps, "accB", [128, 2, 512])
                fin = PS(ps, "finB", [128, 2, 512])
                lam_init = 0.8 - 0.6 * math.exp(-0.3 * l)
                dma(DLB[:], diff_lambda[l:l + 1, :].partition_broadcast(128), [], ['DLB'])
                dv = DLB[:].rearrange("p (a d) -> p a d", a=4)
                tt(DLB[:, 0:64], dv[:, 0, :], dv[:, 1, :], ALU.mult, ['DLB'], ['DLB'])
                tt(DLB[:, 128:192], dv[:, 2, :], dv[:, 3, :], ALU.mult, ['DLB'], ['DLB'])
                S.op('dve', lambda e: e.tensor_reduce(out=lam4[:, 0:1], in_=DLB[:, 0:64], axis=AX.X, op=ALU.add), reads=['DLB'], writes=['lam4'])
                S.op('dve', lambda e: e.tensor_reduce(out=lam4[:, 1:2], in_=DLB[:, 128:192], axis=AX.X, op=ALU.add), reads=['DLB'], writes=['lam4'])
                act(lam4[:, 0:2], lam4[:, 0:2], AF.Exp, ['lam4'], ['lam4'])
                tt(lamn[:], lam4[:, 1:2], lam4[:, 0:1], ALU.subtract, ['lam4'], ['lamn'])
                tsc(lamn[:], lamn[:], -lam_init, None, ALU.add, None, ['lamn'], ['lamn'])
                dma(DG[:], diff_norm_g[l:l + 1, :].rearrange("o e -> e o"), [], ['DG'])
                tsc(DG[:], DG[:], 1.0 - lam_init, None, ALU.mult, None, ['DG'], ['DG'])
                cvb = cfg.col['vb']
                KG = 5

                def b_block(h, q0, nq, kchunks):
                    dma(QB0[:, :nq], PT[2 * H + h][0:64, dsl(q0, nq)], [], ['QB'])
                    dma(QB1[:, :nq], PT[2 * H + h][64:128, dsl(q0, nq)], [], ['QB'])
                    memset(ZA[:], 0.0, ['ZA'])

                    def step(kc, first, lastf):
                        for m in range(2):
                            mm(st_ps[:, m, :nq], KTBm[m][:, dsl(kc * 128, 128)], QBm[m][:, :nq],
                               ['KTB', 'QB'], ['st'])
                        act(PTS[:, :, :nq], st_ps[:, :, :nq], AF.Exp, ['st'], ['PTS'], scale=64 ** -0.5)
                        tt(ZA[:, :, :nq], ZA[:, :, :nq], PTS[:, :, :nq], ALU.add, ['ZA', 'PTS'], ['ZA'])
                        for m in range(2):
                            mm(acc[:, m, :nq], VB[:, dsl(kc, 1), :].rearrange("p o e -> p (o e)"), PTS[:, m, :nq], ['VB', 'PTS'], ['acc'],
                               start=first, stop=lastf)
                    n = len(kchunks)
                    ng = (n + KG - 1) // KG
                    if ng <= 2:
                        for j, kc in enumerate(kchunks):
                            step(kc, j == 0, j == n - 1)
                    else:
                        assert n % KG == 0 and kchunks == list(range(n))
                        for j in range(KG):
                            step(j, j == 0, False)

                        def mid(gidx):
                            for j in range(KG):
                                step(gidx * KG + j, False, False)
                        S.loop(1, ng - 1, mid)
                        for j in range(KG):
                            kc = (ng - 1) * KG + j
                            step(kc, False, j == KG - 1)
                    for m in range(2):
                        mm(fin[:, m, :nq], onesf[:], ZA[:, m, :nq], ['ZA'], ['fin'])
                    S.op('dve', lambda e: e.reciprocal(out=RR[:, :, :nq], in_=fin[:, :, :nq]), reads=['fin'], writes=['RR'])
                    tt(o0[:, :nq], acc[:, 0, :nq], RR[:, 0, :nq], ALU.mult, ['acc', 'RR'], ['o0'])
                    tt(o1[:, :nq], acc[:, 1, :nq], RR[:, 1, :nq], ALU.mult, ['acc', 'RR'], ['o1'])
                    stt(o0[:, :nq], o1[:, :nq], lamn[:, 0:1], o0[:, :nq], ALU.mult, ALU.add, ['o0', 'o1', 'lamn'], ['o0'])
                    tt(o1[:, :nq], o0[:, :nq], o0[:, :nq], ALU.mult, ['o0'], ['o1'])
                    mm(fin[:, 0, :nq], onesf[:], o1[:, :nq], ['o1'], ['fin'])
                    tsc(o1[:, :nq], fin[:, 0, :nq], 1.0 / 128, NORM_EPS, ALU.mult, ALU.add, ['fin'], ['o1'])
                    act(o1[:, :nq], o1[:, :nq], AF.Sqrt, ['o1'], ['o1'])
                    S.op('dve', lambda e: e.reciprocal(out=o1[:, :nq], in_=o1[:, :nq]), reads=['o1'], writes=['o1'])
                    tt(o0[:, :nq], o0[:, :nq], o1[:, :nq], ALU.mult, ['o0', 'o1'], ['o0'])
                    act(obb[:, :nq], o0[:, :nq], AF.Copy, ['o0', 'DG'], ['obb'], scale=DG[:, 0:1])
                    dma(OT[H + h][:, dsl(q0, nq)], obb[:, :nq], ['obb'], ['OT'])

                QBW = min(512, SEQ)
                for h in range(H):
                    S.sync_all()
                    dma(KTB0[:], PT[3 * H + h][0:64, :], [], ['KTB'])
                    dma(KTB1[:], PT[3 * H + h][64:128, :], [], ['KTB'])
                    dma(VB[:], P[:, cvb + h * 128:cvb + (h + 1) * 128].rearrange("(n p) e -> p n e", p=128), [], ['VB'])
                    S.sync_all()
                    nqb = SEQ // QBW
                    if nqb == 1:
                        b_block(h, 0, QBW, list(range(NT)))
                    else:
                        S.sync_all()
                        with S.fori(0, nqb) as qi:
                            b_block(h, qi * QBW, QBW, list(range(NT)))
                            S.sync_all()
                    if with_ctx:
                        b_block(h, SEQ, CTX, list(range(NTL, NT)))
                S.sync_all()

            phase_end('B', l)
            with ExitStack() as ps:
                DEC = SB(ps, "DEC", [128, 2, H])
                DT = SB(ps, "DT", [128, 2, H, 128], BF16)
                QDEC = SB(ps, "QDEC", [128, 2, H])
                KDEC = SB(ps, "KDEC", [128, 2, H])
                CDEC = SB(ps, "CDEC", [128, 2, H])
                PIDX = SB(ps, "PIDX", [128, 1])
                tmpd = SB(ps, "tmpd", [128, 128])
                mskf = SB(ps, "mskf", [128, 128])
                Sst = SB(ps, "Sst", [128, H, 128])
                Sb = SB(ps, "Sb", [128, H, 128], BF16)
                QT = SB(ps, "QTc", [128, H, 128], BF16)
                KT = SB(ps, "KTc", [128, H, 128], BF16)
                Kt = SB(ps, "Ktok", [128, GW], BF16)
                Vt = SB(ps, "Vtok", [128, GW], BF16)
                Gt = SB(ps, "Gtok", [128, GW], BF16)
                Kd = SB(ps, "Kd", [128, 128], BF16)
                ATm = SB(ps, "ATm", [128, 128], BF16)
                tmpo = SB(ps, "tmpo", [128, 128])
                of = SB(ps, "of", [128, GW])
                oft = SB(ps, "oft", [128, GW])
                sg = SB(ps, "sg", [128, GW])
                GNG = SB(ps, "GNG", [128, GW])
                GNB = SB(ps, "GNB", [128, GW])
                st1 = SB(ps, "st1", [128, 1])
                st2 = SB(ps, "st2", [128, 1])
                yob = SB(ps, "yob", [128, GW], BF16)
                oTc = SB(ps, "oTc", [128, H, 128], BF16)
                at_ps = PS(ps, "atC", [128, 128])
                o1_ps = PS(ps, "o1C", [128, 128])
                o2_ps = PS(ps, "o2C", [128, 128])
                m_ps = PS(ps, "mC", [128, 128])
                tp_ps = PS(ps, "tpC", [128, H, 128], BF16)
                dma(DEC[:, 0, :], ret_decay_fwd[l:l + 1, :].partition_broadcast(128), [], ['DEC'])
                dma(DEC[:, 1, :], ret_decay_bwd[l:l + 1, :].partition_broadcast(128), [], ['DEC'])
                dma(GNG[:], ret_gn_g[l:l + 1, :].partition_broadcast(128), [], ['GNG'])
                dma(GNB[:], ret_gn_b[l:l + 1, :].partition_broadcast(128), [], ['GNB'])
                act(DEC[:], DEC[:], AF.Exp, ['DEC'], ['DEC'], scale=-1.0)
                act(DEC[:], DEC[:], AF.Ln, ['DEC'], ['DEC'], bias=1.0)
                tsc(DEC[:], DEC[:], -1.0, None, ALU.mult, None, ['DEC'], ['DEC'])
                tsc(PIDX[:], REL[:, 0:1], -1.0, None, ALU.mult, None, [], ['PIDX'])
                for d in range(2):
                    sgn = 1.0 if d == 0 else -1.0
                    tsc(tmpd[:], REL[:], sgn, 0.0, ALU.mult, ALU.max, [], ['tmpd'])
                    tsc(mskf[:], REL[:], sgn, 0.0, ALU.mult, ALU.is_ge, [], ['mskf'])
                    for h in range(H):
                        act(tmpo[:], tmpd[:], AF.Exp, ['tmpd', 'DEC'], ['tmpo'], scale=DEC[:, d, h:h + 1])
                        tt(DT[:, d, h, :], tmpo[:], mskf[:], ALU.mult, ['tmpo', 'mskf'], ['DT'])
                        if d == 0:
                            tsc(st1[:], PIDX[:], 1.0, None, ALU.add, None, ['PIDX'], ['st1'])
                        else:
                            tsc(st1[:], PIDX[:], -1.0, 128.0, ALU.mult, ALU.add, ['PIDX'], ['st1'])
                        act(QDEC[:, d, h:h + 1], st1[:], AF.Exp, ['st1', 'DEC'], ['QDEC'], scale=DEC[:, d, h:h + 1])
                        if d == 0:
                            tsc(st1[:], PIDX[:], -1.0, 127.0, ALU.mult, ALU.add, ['PIDX'], ['st1'])
                        else:
                            tsc(st1[:], PIDX[:], 1.0, None, ALU.mult, None, ['PIDX'], ['st1'])
                        act(KDEC[:, d, h:h + 1], st1[:], AF.Exp, ['st1', 'DEC'], ['KDEC'], scale=DEC[:, d, h:h + 1])
                act(CDEC[:], DEC[:], AF.Exp, ['DEC'], ['CDEC'], scale=128.0)
                cqc, ckc, cvc, cgc = cfg.col['qc'], cfg.col['kc'], cfg.col['vc'], cfg.col['gc']
                jq, jk = cfg.ptbase['qc'], cfg.ptbase['kc']
                S.sync_all()

                def c_body(i, d):
                    r0 = i * 128
                    dma(QT[:], PTv[:, jq:jq + H, dsl(r0, 128)], [], ['QT'])
                    dma(KT[:], PTv[:, jk:jk + H, dsl(r0, 128)], [], ['KT'])
                    dma(Kt[:], P[dsl(r0, 128), ckc:ckc + GW], [], ['Kt'])
                    dma(Vt[:], P[dsl(r0, 128), cvc:cvc + GW], [], ['Vt'])
                    if d == 1:
                        dma(Gt[:], P[dsl(r0, 128), cgc:cgc + GW], [], ['Gt'])
                        dma(oft[:], OF[dsl(r0, 128), :], [], ['oft'])
                    for h in range(H):
                        hs = slice(h * 128, (h + 1) * 128)
                        mm(at_ps[:], KT[:, h, :], QT[:, h, :], ['KT', 'QT'], ['at'])
                        tt(ATm[:], at_ps[:], DT[:, d, h, :], ALU.mult, ['at'], ['ATm'])
                        mm(o1_ps[:], ATm[:], Vt[:, hs], ['ATm', 'Vt'], ['o1p'])
                        mm(o2_ps[:], QT[:, h, :], Sb[:, h, :], ['QT', 'Sb'], ['o2p'])
                        act(tmpo[:], o2_ps[:], AF.Copy, ['o2p'], ['tmpo'], scale=QDEC[:, d, h:h + 1])
                        tt(of[:, hs], o1_ps[:], tmpo[:], ALU.add, ['o1p', 'tmpo'], ['of'])
                        tsc(Kd[:], Kt[:, hs], KDEC[:, d, h:h + 1], None, ALU.mult, None, ['Kt'], ['Kd'])
                        mm(m_ps[:], Kd[:], Vt[:, hs], ['Kd', 'Vt'], ['mp'])
                        stt(Sst[:, h, :], Sst[:, h, :], CDEC[:, d, h:h + 1], m_ps[:], ALU.mult, ALU.add, ['Sst', 'mp'], ['Sst'])
                        cp(Sb[:, h, :], Sst[:, h, :], ['Sst'], ['Sb'], eng='act')
                    if d == 0:
                        dma(OF[dsl(r0, 128), :], of[:], ['of'], ['OF'])
                    else:
                        tt(of[:], of[:], oft[:], ALU.add, ['of', 'oft'], ['of'])
                        for h in range(H):
                            hs = slice(h * 128, (h + 1) * 128)
                            S.op('dve', lambda e: e.tensor_reduce(out=st1[:], in_=of[:, hs], axis=AX.X, op=ALU.add), reads=['of'], writes=['st1'])
                            tsc(st1[:], st1[:], -1.0 / 128, None, ALU.mult, None, ['st1'], ['st1'])
                            tsc(of[:, hs], of[:, hs], st1[:, 0:1], None, ALU.add, None, ['of', 'st1'], ['of'])
                            act(tmpo[:], of[:, hs], AF.Square, ['of'], ['tmpo', 'st2'], accum_out=st2[:])
                            rstd_from(st2[:], 128, GN_EPS, 'st2')
                            tsc(of[:, hs], of[:, hs], st2[:, 0:1], None, ALU.mult, None, ['of', 'st2'], ['of'])
                        tt(of[:], of[:], GNG[:], ALU.mult, ['of'], ['of'])
                        tt(of[:], of[:], GNB[:], ALU.add, ['of'], ['of'])
                        act(sg[:], Gt[:], AF.Silu, ['Gt'], ['sg'])
                        tt(yob[:], of[:], sg[:], ALU.mult, ['of', 'sg'], ['yob'])
                        for h in range(H):
                            tr(tp_ps[:, h, :], yob[:, h * 128:(h + 1) * 128], ident[:], ['yob'], ['tp'])
                        cp(oTc[:], tp_ps[:], ['tp'], ['oTc'])
                        dma(OTv[:, 2 * H:3 * H, dsl(r0, 128)], oTc[:], ['oTc'], ['OT'])

                for d in range(2):
                    memset(Sst[:], 0.0, ['Sst'])
                    memset(Sb[:], 0.0, ['Sb'])
                    ctx_order = list(range(NTL, NT)) if d == 0 else list(range(NT - 1, NTL - 1, -1))
                    for i in ctx_order:
                        c_body(i, d)
                    if d == 0:
                        S.loop(0, NTL, lambda i: c_body(i, 0))
                    else:
                        S.loop(0, NTL, lambda i: c_body(NTL - 1 - i, 1))
                    S.sync_all()

            phase_end('C', l)
            with ExitStack() as ps:
                WOB = SB(ps, "WOB", [128, 4 * H, D], BF16)
                wof = SB(ps, "wof", [128, D])
                G1B = SB(ps, "G1B", [128, D])
                GM2B = SB(ps, "GM2B", [128, D])
                SH2B = SB(ps, "SH2B", [128, D])
                N2B = SB(ps, "N2B", [128, D])
                xt = SB(ps, "xtF", [128, D])
                x1 = SB(ps, "x1F", [128, D])
                h2f = SB(ps, "h2f", [128, D])
                h2b = SB(ps, "h2b", [128, D], BF16)
                junk = SB(ps, "junkF", [128, D], BF16)
                oT = SB(ps, "oTF", [128, 4 * H, 128], BF16)
                h2T = SB(ps, "h2T", [128, KC, 128], BF16)
                WRB = SB(ps, "WRB", [128, KC, NE], BF16)
                wrf = SB(ps, "wrf", [128, KC, NE])
                ss = SB(ps, "ssF", [128, 1])
                lg = SB(ps, "lgF", [128, NE])
                mx = SB(ps, "mxF", [128, 1])
                sm = SB(ps, "smF", [128, 1])
                affs = SB(ps, "affs", [128, NE])
                po = PS(ps, "poF", [128, 512])
                pst = PS(ps, "pstF", [128, KC, 128], BF16)
                pr = PS(ps, "prF", [128, NE])
                for c in range(4 * H):
                    dma(wof[:], w_out[l][c * 128:(c + 1) * 128, :], [], ['wof'])
                    cp(WOB[:, c, :], wof[:], ['wof'], ['WOB'], eng=('dve' if c % 2 == 0 else 'act'))
                dma(wrf[:], w_router[l].rearrange("(kc p) e -> p kc e", p=128), [], ['wrf'])
                cp(WRB[:], wrf[:], ['wrf'], ['WRB'])
                dma(N2B[:], norm2_g[l:l + 1, :].partition_broadcast(128), [], ['N2B'])

                def load_mod2(r):
                    dma(G1B[:], MODV[r:r + 1, 2 * D:3 * D].partition_broadcast(128), [], ['G1B'])
                    dma(GM2B[:], MODV[r:r + 1, 4 * D:5 * D].partition_broadcast(128), [], ['GM2B'])
                    dma(SH2B[:], MODV[r:r + 1, 3 * D:4 * D].partition_broadcast(128), [], ['SH2B'])
                    stt(GM2B[:], GM2B[:], 1.0, N2B[:], ALU.add, ALU.mult, ['GM2B', 'N2B'], ['GM2B'])

                def f_body(i):
                    r0 = i * 128
                    dma(oT[:], OTv[:, :, dsl(r0, 128)], [], ['oT'])
                    dma(xt[:], src[dsl(r0, 128), :], [], ['xt'])
                    for n in range(ND):
                        ns = slice(n * NW, (n + 1) * NW)
                        for c in range(4 * H):
                            mm(po[:, :NW], oT[:, c, :], WOB[:, c, ns], ['oT', 'WOB'], ['po'], start=(c == 0), stop=(c == 4 * H - 1))
                        tt(x1[:, ns], po[:, :NW], G1B[:, ns], ALU.mult, ['po', 'G1B'], ['x1'])
                    tt(x1[:], x1[:], xt[:], ALU.add, ['x1', 'xt'], ['x1'])
                    dma(X[dsl(r0, 128), :], x1[:], ['x1'], ['X'])
                    act(junk[:], x1[:], AF.Square, ['x1'], ['junk', 'ss'], accum_out=ss[:])
                    rstd_from(ss[:], D, NORM_EPS, 'ss')
                    stt(h2f[:], x1[:], ss[:, 0:1], GM2B[:], ALU.mult, ALU.mult, ['x1', 'ss', 'GM2B'], ['h2f'])
                    tt(h2b[:], h2f[:], SH2B[:], ALU.add, ['h2f', 'SH2B'], ['h2b'])
                    dma(H2[dsl(r0, 128), :], h2b[:], ['h2b'], ['H2'])
                    for kc in range(KC):
                        tr(pst[:, kc, :], h2b[:, kc * 128:(kc + 1) * 128], ident[:], ['h2b'], ['pst'])
                    cp(h2T[:], pst[:], ['pst'], ['h2T'])
                    for kc in range(KC):
                        mm(pr[:], h2T[:, kc, :], WRB[:, kc, :], ['h2T', 'WRB'], ['pr'], start=(kc == 0), stop=(kc == KC - 1))
                    cp(lg[:], pr[:], ['pr'], ['lg'])
                    S.op('dve', lambda e: e.tensor_reduce(out=mx[:], in_=lg[:], axis=AX.X, op=ALU.max), reads=['lg'], writes=['mx'])
                    tsc(mx[:], mx[:], -1.0, None, ALU.mult, None, ['mx'], ['mx'])
                    act(affs[:], lg[:], AF.Exp, ['lg', 'mx'], ['affs', 'sm'], bias=mx[:, 0:1], accum_out=sm[:])
                    recip(sm[:], sm[:], ['sm'], ['sm'])
                    tsc(AFFT[:, dsl(i, 1), :].rearrange("p o e -> p (o e)"), affs[:], sm[:, 0:1], None, ALU.mult, None, ['affs', 'sm'], ['AFFT'])

                load_mod2(0)
                S.loop(0, NTL, f_body)
                S.sync_all()
                if with_ctx:
                    load_mod2(1)
                    for i in range(NTL, NT):
                        f_body(i)
                    S.sync_all()

            phase_end('F', l)
            groups = [(0, NTL, CAP)] + ([(NTL, NT, CAPC)] if with_ctx else [])
            with ExitStack() as ps:
                THR = SB(ps, "THR", [128, 2, NE])
                HI = SB(ps, "HI", [128, 2, NE])
                MID = SB(ps, "MID", [128, 2, NE])
                cmpt = SB(ps, "cmpt", [128, NTL])
                cnt = SB(ps, "cnt", [128, 2, NE])
                gef = SB(ps, "gef", [128, 2, NE])
                dlt = SB(ps, "dlt", [128, 2, NE])
                OFF = SB(ps, "OFF", [128, NE])
                mk = SB(ps, "mkM", [128, NE])
                mkb = SB(ps, "mkbM", [128, NE], BF16)
                t1 = SB(ps, "t1M", [128, NE])
                posc = SB(ps, "posc", [128, NE], I32)
                idx1 = SB(ps, "idx1", [128, 1], I32)
                h2t = SB(ps, "h2tM", [128, D], BF16)
                tot = PS(ps, "totM", [128, 2, NE])
                cum = PS(ps, "cumM", [128, NE])
                tot2 = PS(ps, "tot2M", [128, NE])
                memset(THR[:], 0.0, ['THR'])
                memset(HI[:], 1.0, ['HI'])
                memset(MID[:], 0.5, ['MID'])
                memset(cnt[:], 0.0, ['cnt'])
                S.sync_all()

                def bis_body(it):
                    for gi, (t0, t1_, cap) in enumerate(groups):
                        for e in range(NE):
                            tsc(cmpt[:, :t1_ - t0], AFFT[:, t0:t1_, e], MID[:, gi, e:e + 1], None, ALU.is_ge, None, ['MID'], ['cmpt'])
                            S.op('dve', lambda en: en.tensor_reduce(out=cnt[:, gi, e:e + 1], in_=cmpt[:, :t1_ - t0], axis=AX.X, op=ALU.add),
                                 reads=['cmpt'], writes=['cnt'])
                    mm(tot[:].rearrange("p a e -> p (a e)"), onesf[:], cnt[:].rearrange("p a e -> p (a e)"), ['cnt'], ['tot'])
                    for gi, (t0, t1_, cap) in enumerate(groups):
                        tsc(gef[:, gi, :], tot[:, gi, :], float(cap) - 0.5, None, ALU.is_ge, None, ['tot'], ['gef'])
                    if len(groups) == 1:
                        memset(gef[:, 1, :], 0.0, ['gef'])
                    tt(dlt[:], MID[:], THR[:], ALU.subtract, ['MID', 'THR'], ['dlt'])
                    tt(dlt[:], dlt[:], gef[:], ALU.mult, ['dlt', 'gef'], ['dlt'])
                    tt(THR[:], THR[:], dlt[:], ALU.add, ['THR', 'dlt'], ['THR'])
                    tt(dlt[:], HI[:], MID[:], ALU.subtract, ['HI', 'MID'], ['dlt'])
                    tt(dlt[:], dlt[:], gef[:], ALU.mult, ['dlt', 'gef'], ['dlt'])
                    tt(HI[:], MID[:], dlt[:], ALU.add, ['MID', 'dlt'], ['HI'])
                    tt(MID[:], THR[:], HI[:], ALU.add, ['THR', 'HI'], ['MID'])
                    tsc(MID[:], MID[:], 0.5, None, ALU.mult, None, ['MID'], ['MID'])
                S.loop(0, 40, bis_body)
                S.sync_all()
                memset(OFF[:], 0.0, ['OFF'])
                BIG = float(NE * CAPT + 64)

                def m2_body(i, gi):
                    r0 = i * 128
                    a = AFFT[:, dsl(i, 1), :].rearrange("p o e -> p (o e)")
                    tt(mk[:], a, THR[:, gi, :], ALU.is_ge, ['AFFT', 'THR'], ['mk'])
                    cp(mkb[:], mk[:], ['mk'], ['mkb'])
                    mm(cum[:], TRIU[:], mkb[:], ['mkb'], ['cum'])
                    mm(tot2[:], onesb[:], mkb[:], ['mkb'], ['tot2'])
                    tt(t1[:], cum[:], OFF[:], ALU.add, ['cum', 'OFF'], ['t1'])
                    tt(t1[:], t1[:], EOFF[:], ALU.add, ['t1'], ['t1'])
                    tsc(t1[:], t1[:], -1.0 - BIG, None, ALU.add, None, ['t1'], ['t1'])
                    tt(t1[:], t1[:], mk[:], ALU.mult, ['t1', 'mk'], ['t1'])
                    tsc(t1[:], t1[:], BIG, None, ALU.add, None, ['t1'], ['t1'])
                    cp(posc[:], t1[:], ['t1'], ['posc'])
                    cp(POSI[:, dsl(i, 1), :].rearrange("p o e -> p (o e)"), posc[:], ['posc'], ['POSI'])
                    tt(WGT[:, dsl(i, 1), :].rearrange("p o e -> p (o e)"), a, mk[:], ALU.mult, ['AFFT', 'mk'], ['WGT'])
                    tt(OFF[:], OFF[:], tot2[:], ALU.add, ['OFF', 'tot2'], ['OFF'])
                    dma(h2t[:], H2[dsl(r0, 128), :], [], ['h2t'])
                    for e in range(NE if not getattr(cfg, 'no_scatter', False) else 0):
                        cp(idx1[:], posc[:, e:e + 1], ['posc'], ['idx1'])
                        ind_dma(lambda en: en.indirect_dma_start(
                            out=XEf[:, :], out_offset=bass.IndirectOffsetOnAxis(ap=idx1[:, :], axis=0),
                            in_=h2t[:, :], in_offset=None, bounds_check=NE * CAPT - 1, oob_is_err=False),
                            NE * CAPT - 1, ['h2t', 'idx1'], ['XE'])

                for i in range(NTL):
                    m2_body(i, 0)
                    if i % 16 == 15:
                        S.sync_all()
                S.sync_all()
                if with_ctx:
                    for i in range(NTL, NT):
                        m2_body(i, 1)
                    S.sync_all()

            phase_end('M2', l)
            with ExitStack() as ps:
                WG = SB(ps, "WG", [128, KC, FF], BF16)
                WU = SB(ps, "WU", [128, KC, FF], BF16)
                WD = SB(ps, "WD", [128, FC, D], BF16)
                stg = SB(ps, "stg", [128, 4096])
                xe = SB(ps, "xe", [128, D], BF16)
                xT = SB(ps, "xT", [128, KC, 512], BF16)
                hT = SB(ps, "hTM", [128, FC, 512], BF16)
                sgt = SB(ps, "sgt", [128, 512])
                yt = SB(ps, "ytM", [128, D], BF16)
                pst = PS(ps, "pstM", [128, KC, 128], BF16)
                pg = PS(ps, "pgM", [128, 512])
                pu = PS(ps, "puM", [128, 512])
                pd = PS(ps, "pdM", [128, 512])
                blocks = [(s0, min(512, CAP - s0)) for s0 in range(0, CAP, 512)]
                if with_ctx:
                    blocks += [(CAP + s0, min(512, CAPC - s0)) for s0 in range(0, CAPC, 512)]
                kstep = max(1, 4096 // FF)
                fstep = max(1, 4096 // D)
                wgl, wul, wdl = w_gate[l], w_up[l], w_down[l]
                cnt_eng = [0]

                def conv(out, in_, R, W):
                    eng = ('dve', 'act', 'pool')[cnt_eng[0] % 3]
                    cnt_eng[0] += 1
                    cp(out, in_, R, W, eng=eng)

                def m3_body(e):
                    for (wsrc, wdst) in ((wgl, WG), (wul, WU)):
                        wv = wsrc[dsl(e, 1)].rearrange("o (kc p) f -> p (o kc) f", p=128)
                        for k0 in range(0, KC, kstep):
                            dma(stg[:, :kstep * FF].rearrange("p (k f) -> p k f", f=FF), wv[:, k0:k0 + kstep, :], [], ['stg'])
                            conv(wdst[:, k0:k0 + kstep, :], stg[:, :kstep * FF].rearrange("p (k f) -> p k f", f=FF), ['stg'], ['W'])
                    wv = wdl[dsl(e, 1)].rearrange("o (fc p) n -> p (o fc) n", p=128)
                    for f0 in range(0, FC, fstep):
                        dma(stg[:, :fstep * D].rearrange("p (k f) -> p k f", f=D), wv[:, f0:f0 + fstep, :], [], ['stg'])
                        conv(WD[:, f0:f0 + fstep, :], stg[:, :fstep * D].rearrange("p (k f) -> p k f", f=D), ['stg'], ['W'])
                    XEe = XE[dsl(e, 1)].rearrange("o s d -> (o s) d")
                    YEe = YE[dsl(e, 1)].rearrange("o s d -> (o s) d")
                    for (s0, nb) in blocks:
                        for st0 in range(0, nb, 128):
                            ns_ = min(128, nb - st0)
                            dma(xe[:ns_, :], XEe[s0 + st0:s0 + st0 + ns_, :], [], ['xe'])
                            for kc in range(KC):
                                tr(pst[:, kc, :ns_], xe[:ns_, kc * 128:(kc + 1) * 128], ident[:ns_, :ns_], ['xe'], ['pst'])
                            cp(xT[:, :, st0:st0 + ns_], pst[:, :, :ns_], ['pst'], ['xT'])
                        for fc in range(FC):
                            for kc in range(KC):
                                mm(pg[:, :nb], WG[:, kc, fc * 128:(fc + 1) * 128], xT[:, kc, :nb], ['W', 'xT'], ['pg'], start=(kc == 0), stop=(kc == KC - 1))
                            for kc in range(KC):
                                mm(pu[:, :nb], WU[:, kc, fc * 128:(fc + 1) * 128], xT[:, kc, :nb], ['W', 'xT'], ['pu'], start=(kc == 0), stop=(kc == KC - 1))
                            act(sgt[:, :nb], pg[:, :nb], AF.Silu, ['pg'], ['sgt'])
                            tt(hT[:, fc, :nb], sgt[:, :nb], pu[:, :nb], ALU.mult, ['sgt', 'pu'], ['hTM'])
                        for st0 in range(0, nb, 128):
                            ns_ = min(128, nb - st0)
                            for n in range(ND):
                                nsl = slice(n * NW, (n + 1) * NW)
                                for fc in range(FC):
                                    mm(pd[:ns_, :NW], hT[:, fc, st0:st0 + ns_], WD[:, fc, nsl], ['hTM', 'W'], ['pd'], start=(fc == 0), stop=(fc == FC - 1))
                                cp(yt[:ns_, nsl], pd[:ns_, :NW], ['pd'], ['yt'], eng=('act' if n % 2 else 'dve'))
                            dma(YEe[s0 + st0:s0 + st0 + ns_, :], yt[:ns_, :], ['yt'], ['YE'])
                S.loop(0, NE, m3_body)
                S.sync_all()

            phase_end('M3', l)
            with ExitStack() as ps:
                G2B = SB(ps, "G2B", [128, D])
                FNB = SB(ps, "FNB", [128, D])
                x1 = SB(ps, "x1C", [128, D])
                accm = SB(ps, "accm", [128, D])
                Gg = SB(ps, "Gg", [128, D], BF16)
                posc = SB(ps, "poscC", [128, NE], I32)
                idx1c = SB(ps, "idx1c", [128, 1], I32)
                wcc = SB(ps, "wcc", [128, NE])
                junk = SB(ps, "junkC", [128, D], BF16)
                ss = SB(ps, "ssC", [128, 1])
                memset(Gg[:], 0.0, ['Gg'])
                if last:
                    dma(FNB[:], final_norm_g[0:1, :].partition_broadcast(128), [], ['FNB'])

                def m4_body(i):
                    r0 = i * 128
                    dma(x1[:], X[dsl(r0, 128), :], ['X'], ['x1'])
                    cp(posc[:], POSI[:, dsl(i, 1), :].rearrange("p o e -> p (o e)"), ['POSI'], ['posc'])
                    cp(wcc[:], WGT[:, dsl(i, 1), :].rearrange("p o e -> p (o e)"), ['WGT'], ['wcc'])
                    memset(accm[:], 0.0, ['accm'])
                    for e in range(NE):
                        cp(idx1c[:], posc[:, e:e + 1], ['posc'], ['idx1c'])
                        ind_dma(lambda en: en.indirect_dma_start(
                            out=Gg[:, :], out_offset=None, in_=YEf[:, :],
                            in_offset=bass.IndirectOffsetOnAxis(ap=idx1c[:, :], axis=0),
                            bounds_check=NE * CAPT - 1, oob_is_err=False), NE * CAPT - 1, ['idx1c', 'YE', 'Gg'], ['Gg'])
                        stt(accm[:], Gg[:], wcc[:, e:e + 1], accm[:], ALU.mult, ALU.add, ['Gg', 'wcc', 'accm'], ['accm'])
                    tt(accm[:], accm[:], G2B[:], ALU.mult, ['accm', 'G2B'], ['accm'])
                    tt(x1[:], x1[:], accm[:], ALU.add, ['x1', 'accm'], ['x1'])
                    if not last:
                        dma(X[dsl(r0, 128), :], x1[:], ['x1'], ['X'])
                    else:
                        act(junk[:], x1[:], AF.Square, ['x1'], ['junk', 'ss'], accum_out=ss[:])
                        rstd_from(ss[:], D, NORM_EPS, 'ss')
                        stt(accm[:], x1[:], ss[:, 0:1], FNB[:], ALU.mult, ALU.mult, ['x1', 'ss', 'FNB'], ['accm'])
                        dma(yout[dsl(r0, 128), :], accm[:], ['accm'], ['yout'])

                dma(G2B[:], MODV[0:1, 5 * D:6 * D].partition_broadcast(128), [], ['G2B'])
                for i in range(NTL):
                    m4_body(i)
                    if i % 16 == 15:
                        S.sync_all()
                S.sync_all()
                if with_ctx:
                    dma(G2B[:], MODV[1:2, 5 * D:6 * D].partition_broadcast(128), [], ['G2B'])
                    for i in range(NTL, NT):
                        m4_body(i)
                    S.sync_all()

            phase_end('M4', l)
          except StopBuild:
            S.sync_all()
            for nm, (src_ap, dst_ap) in dbg.items():
                dma(dst_ap, src_ap, [], ['dbg' + nm])
            S.sync_all()
            break
        S.sync_all()
        print("instructions:", S.ninst)
    return nc


def rope_tables(cfg):
    SEQ, GW = cfg.SEQ, cfg.GW
    t = np.arange(SEQ)
    row = (t // GRID_W).astype(np.float32)
    col = (t % GRID_W).astype(np.float32)
    out = np.zeros((SEQ, 4 * GW), np.float32)
    for k, dim in enumerate((64, 128)):
        nf = dim // 4
        inv = (ROPE_BASE ** (-np.arange(nf, dtype=np.float32) / nf)).astype(np.float32)
        ar = (row[:, None] * inv).astype(np.float32)
        ac = (col[:, None] * inv).astype(np.float32)
        cosh = np.concatenate([np.cos(ar), np.cos(ar), np.cos(ac), np.cos(ac)], axis=1)
        sinh = np.concatenate([-np.sin(ar), np.sin(ar), -np.sin(ac), np.sin(ac)], axis=1)
        nh = GW // dim
        out[:, (2 * k) * GW:(2 * k + 1) * GW] = np.tile(cosh, (1, nh))
        out[:, (2 * k + 1) * GW:(2 * k + 2) * GW] = np.tile(sinh, (1, nh))
    return out.astype(np.float32)


def na_bias_tables(cfg, rpb):
    L, H, NTL = cfg.L, cfg.H, cfg.NTL
    rows = NTL * 2
    cols = np.arange(GRID_W)
    cstart = np.clip(cols - 8, 0, GRID_W - 16)
    kc = cols[:, None]
    qc = cols[None, :]
    ok = (kc >= cstart[None, :]) & (kc < cstart[None, :] + 16)
    co = np.clip(kc - qc + 15, 0, 30)
    out = np.full((L, cfg.NTYPE, H, 128, 5, 128), NEG_INF, np.float32)
    types = [2, 0, 1, NTL - 2, NTL - 1]
    for ti, i in enumerate(types):
        base = min(max(i - 2, 0), NTL - 5)
        for c in range(5):
            kt = base + c
            for kr2 in range(2):
                krow = 2 * kt + kr2
                for qr2 in range(2):
                    r = 2 * i + qr2
                    st = min(max(r - 4, 0), rows - 8)
                    if st <= krow < st + 8:
                        ro = krow - r + 7
                        vals = rpb[:, :, ro, :][:, :, co]
                        vals = np.where(ok[None, None], vals, np.float32(NEG_INF))
                        out[:, ti, :, kr2 * 64:(kr2 + 1) * 64, c, qr2 * 64:(qr2 + 1) * 64] = vals
    out = out.transpose(0, 3, 1, 2, 4, 5).reshape(L, 128, cfg.NTYPE * H * 5 * 128)
    return np.ascontiguousarray(out)


def make_in_maps(cfg, inp):
    B = inp['x'].shape[0]
    rope = rope_tables(cfg)
    nab = na_bias_tables(cfg, np.asarray(inp['na_rpb'], np.float32))
    maps = []
    for b in range(B):
        cl = np.stack([np.asarray(inp['c'][b]).reshape(cfg.KC, 128).T, np.asarray(inp['c_ctx']).reshape(cfg.KC, 128).T], axis=-1)
        m = {
            'xin': np.ascontiguousarray(np.concatenate([inp['x'][b], inp['ctx'][b]], axis=0)),
            'cl': np.ascontiguousarray(cl.reshape(128, cfg.KC * 2)).astype(np.float32),
            'nab': nab, 'rope': rope,
            'diff_lambda': np.asarray(inp['diff_lambda']).reshape(cfg.L, 256),
            'final_norm_g': np.asarray(inp['final_norm_g']).reshape(1, cfg.D),
        }
        for k in ('w_mod', 'b_mod', 'norm1_g', 'w_in', 'diff_norm_g', 'ret_decay_fwd', 'ret_decay_bwd', 'ret_gn_g',
                  'ret_gn_b', 'swa_sink', 'w_out', 'norm2_g', 'w_router', 'w_gate', 'w_up', 'w_down'):
            m[k] = np.asarray(inp[k], np.float32)
        maps.append(m)
    return maps


def kernel(**inputs):
    cfg = Cfg()
    nc = build_program(cfg)
    maps = make_in_maps(cfg, inputs)
    res = run_bass_kernel_spmd(nc, maps, core_ids=list(range(len(maps))))
    return np.stack([np.asarray(r['y']) for r in res.results], axis=0).astype(np.float32)
```
